# Optimizing a Trainium2 kernel written in Bass

```python
import math
import jax
import jax.numpy as jnp
from jax import lax
import numpy as np

D_MODEL = 2048
BATCH = 4
SEQ = 4096
DEPTH = 2

F32 = jnp.float32
RMS_EPS = 1e-6
N_BRANCH = 3
BRANCH_WIDTH = D_MODEL // 2
DN_HEAD_DIM = 128
DN_HEADS = BRANCH_WIDTH // DN_HEAD_DIM
DN_WIDTH = DN_HEADS * DN_HEAD_DIM
DN_CONV = 4
DN_CHUNK = 64
FOX_HEAD_DIM = 128
FOX_HEADS = BRANCH_WIDTH // FOX_HEAD_DIM
FOX_WIDTH = FOX_HEADS * FOX_HEAD_DIM
Q_BLOCK = 128
SWA_HEAD_DIM = 64
SWA_Q_HEADS = BRANCH_WIDTH // SWA_HEAD_DIM
SWA_GROUP = 8
SWA_KV_HEADS = SWA_Q_HEADS // SWA_GROUP
SWA_Q_WIDTH = SWA_Q_HEADS * SWA_HEAD_DIM
SWA_KV_WIDTH = SWA_KV_HEADS * SWA_HEAD_DIM
SWA_WINDOW = 128
ROPE_THETA = 10000.0
MAX_POS_OFFSET = 2048
D_FF = 7 * D_MODEL // 2
N_EXPERTS = 8
TOP_K = 2
N_DENSE = (DEPTH + 1) // 2
N_MOE = DEPTH // 2
IN_SPLIT_WIDTHS = (DN_WIDTH, DN_WIDTH, DN_WIDTH, DN_WIDTH, DN_HEADS, DN_HEADS,
                   FOX_WIDTH, FOX_WIDTH, FOX_WIDTH, FOX_HEADS,
                   SWA_Q_WIDTH, SWA_KV_WIDTH, SWA_KV_WIDTH,
                   N_BRANCH * D_MODEL)
IN_WIDTH = sum(IN_SPLIT_WIDTHS)

kernel_name = "hybrid_gdn_fox_swa_moe_adaln"


def rms_norm(x, gain):
    xf = x.astype(F32)
    y = xf * lax.rsqrt(jnp.mean(xf * xf, axis=-1, keepdims=True) + RMS_EPS)
    return (y * gain.astype(F32)).astype(x.dtype)


def l2_normalize(x):
    return x * lax.rsqrt(jnp.sum(x * x, axis=-1, keepdims=True) + RMS_EPS)


def modulate(h, shift, scale):
    return h * (1 + scale[:, None, :]) + shift[:, None, :]


def causal_depthwise_conv(x, w):
    k_width, ch = w.shape
    return lax.conv_general_dilated(x, w[:, None, :].astype(x.dtype), window_strides=(1,),
                                    padding=[(k_width - 1, 0)],
                                    dimension_numbers=('NWC', 'WIO', 'NWC'),
                                    feature_group_count=ch)


def rope(x, cos, sin):
    x1, x2 = jnp.split(x.astype(F32), 2, axis=-1)
    return jnp.concatenate([x1 * cos - x2 * sin, x2 * cos + x1 * sin], axis=-1).astype(x.dtype)


def chunk_gated_delta_rule(q, k, v, g, beta):
    bsz, seq, heads, dk = q.shape
    dv = v.shape[-1]
    n_chunks = seq // DN_CHUNK

    def to_chunks(t):
        return t.reshape(bsz, n_chunks, DN_CHUNK, heads, -1).transpose(1, 0, 3, 2, 4)

    qc, kc, vc = to_chunks(q), to_chunks(k), to_chunks(v)
    gc = jnp.cumsum(g.reshape(bsz, n_chunks, DN_CHUNK, heads).transpose(1, 0, 3, 2), axis=-1)
    bc = beta.reshape(bsz, n_chunks, DN_CHUNK, heads).transpose(1, 0, 3, 2)[..., None]
    causal = jnp.tril(jnp.ones((DN_CHUNK, DN_CHUNK), dtype=bool))
    strict = jnp.tril(jnp.ones((DN_CHUNK, DN_CHUNK), dtype=bool), k=-1)
    decay = jnp.exp(jnp.where(causal, gc[..., :, None] - gc[..., None, :], -jnp.inf))
    kb = kc * bc
    lower = jnp.where(strict, jnp.einsum('nbhcd,nbhed->nbhce', kb, kc) * decay, 0.0)
    rhs = jnp.concatenate([vc * bc, kb * jnp.exp(gc)[..., None]], axis=-1)
    sol = lax.linalg.triangular_solve(lower, rhs, left_side=True, lower=True, unit_diagonal=True)
    u, w = sol[..., :dv], sol[..., dv:]
    qk = jnp.einsum('nbhcd,nbhed->nbhce', qc, kc) * decay
    q_dec = qc * jnp.exp(gc)[..., None]
    k_dec = kc * jnp.exp(gc[..., -1:] - gc)[..., None]
    g_last = jnp.exp(gc[..., -1])

    def step(state, inp):
        qk_i, qd_i, kd_i, u_i, w_i, gl_i = inp
        v_new = u_i - jnp.einsum('bhcd,bhdv->bhcv', w_i, state)
        o_i = jnp.einsum('bhcd,bhdv->bhcv', qd_i, state) + jnp.einsum('bhce,bhev->bhcv', qk_i, v_new)
        state = state * gl_i[..., None, None] + jnp.einsum('bhcd,bhcv->bhdv', kd_i, v_new)
        return state, o_i

    state0 = jnp.zeros((bsz, heads, dk, dv), F32)
    _, o = lax.scan(step, state0, (qk, q_dec, k_dec, u, w, g_last))
    return o.transpose(1, 0, 3, 2, 4).reshape(bsz, seq, heads, dv)


def gated_deltanet(q, k, v, z, beta_logit, a_logit, conv_w, a_log, dt_bias, norm_gain):
    bsz, seq, _ = q.shape
    qkv = jax.nn.silu(causal_depthwise_conv(jnp.concatenate([q, k, v], axis=-1), conv_w)).astype(F32)
    q, k, v = jnp.split(qkv, 3, axis=-1)
    q = l2_normalize(q.reshape(bsz, seq, DN_HEADS, DN_HEAD_DIM)) * (DN_HEAD_DIM ** -0.5)
    k = l2_normalize(k.reshape(bsz, seq, DN_HEADS, DN_HEAD_DIM))
    v = v.reshape(bsz, seq, DN_HEADS, DN_HEAD_DIM)
    beta = jax.nn.sigmoid(beta_logit.astype(F32))
    g = -jnp.exp(a_log.astype(F32)) * jax.nn.softplus(a_logit.astype(F32) + dt_bias.astype(F32))
    o = chunk_gated_delta_rule(q, k, v, g, beta)
    zf = z.astype(F32).reshape(bsz, seq, DN_HEADS, DN_HEAD_DIM)
    o = rms_norm(o, norm_gain) * jax.nn.silu(zf)
    return o.reshape(bsz, seq, DN_WIDTH).astype(z.dtype)


def forgetting_attention(q, k, v, f_logit, b_forget):
    bsz, seq, _ = q.shape
    q = q.reshape(bsz, seq, FOX_HEADS, FOX_HEAD_DIM)
    k = k.reshape(bsz, seq, FOX_HEADS, FOX_HEAD_DIM)
    v = v.reshape(bsz, seq, FOX_HEADS, FOX_HEAD_DIM)
    log_f = jax.nn.log_sigmoid(f_logit.astype(F32) + b_forget.astype(F32))
    cum = jnp.cumsum(log_f, axis=1).transpose(0, 2, 1)
    n_blocks = seq // Q_BLOCK
    q_blocks = q.reshape(bsz, n_blocks, Q_BLOCK, FOX_HEADS, FOX_HEAD_DIM).transpose(1, 0, 2, 3, 4)
    c_blocks = cum.reshape(bsz, FOX_HEADS, n_blocks, Q_BLOCK).transpose(2, 0, 1, 3)
    k_pos = jnp.arange(seq)
    scale = FOX_HEAD_DIM ** -0.5

    def block(args):
        idx, q_i, c_i = args
        s = jnp.einsum('bqhd,bkhd->bhqk', q_i, k).astype(F32) * scale
        s = s + c_i[..., :, None] - cum[:, :, None, :]
        q_pos = idx * Q_BLOCK + jnp.arange(Q_BLOCK)
        s = jnp.where(k_pos[None, :] <= q_pos[:, None], s, -jnp.inf)
        p = jax.nn.softmax(s, axis=-1).astype(v.dtype)
        return jnp.einsum('bhqk,bkhd->bqhd', p, v)

    o = lax.map(block, (jnp.arange(n_blocks), q_blocks, c_blocks))
    return o.transpose(1, 0, 2, 3, 4).reshape(bsz, seq, FOX_WIDTH)


def sliding_window_attention(q, k, v, sinks, cos, sin):
    bsz, seq, _ = q.shape
    win = SWA_WINDOW
    n_blocks = seq // win
    q = rope(q.reshape(bsz, seq, SWA_Q_HEADS, SWA_HEAD_DIM), cos, sin)
    k = rope(k.reshape(bsz, seq, SWA_KV_HEADS, SWA_HEAD_DIM), cos, sin)
    v = v.reshape(bsz, seq, SWA_KV_HEADS, SWA_HEAD_DIM)
    qb = q.reshape(bsz, n_blocks, win, SWA_KV_HEADS, SWA_GROUP, SWA_HEAD_DIM)

    def band(t):
        tp = jnp.pad(t, ((0, 0), (win, 0), (0, 0), (0, 0))).reshape(bsz, n_blocks + 1, win, SWA_KV_HEADS, SWA_HEAD_DIM)
        return jnp.concatenate([tp[:, :-1], tp[:, 1:]], axis=2)

    kb, vb = band(k), band(v)
    s = jnp.einsum('bnqhgd,bnkhd->bnhgqk', qb, kb).astype(F32) * (SWA_HEAD_DIM ** -0.5)
    q_off = jnp.arange(win)
    k_off = jnp.arange(2 * win) - win
    rel = q_off[:, None] - k_off[None, :]
    k_abs = jnp.arange(n_blocks)[:, None] * win + k_off[None, :]
    mask = ((rel >= 0) & (rel < win))[None] & (k_abs >= 0)[:, None, :]
    s = jnp.where(mask[None, :, None, None], s, -jnp.inf)
    sink = jnp.broadcast_to(sinks.astype(F32).reshape(SWA_KV_HEADS, SWA_GROUP)[None, None, :, :, None, None],
                            s.shape[:-1] + (1,))
    p = jax.nn.softmax(jnp.concatenate([s, sink], axis=-1), axis=-1)[..., :2 * win]
    o = jnp.einsum('bnhgqk,bnkhd->bnqhgd', p.astype(v.dtype), vb)
    return o.reshape(bsz, seq, SWA_Q_WIDTH)


def token_mixer(h, cos, sin, w_in, conv_w, a_log, dt_bias, dn_norm, b_forget, sinks, w_branch, w_out):
    bsz, seq, _ = h.shape
    offsets = tuple(int(o) for o in np.cumsum(IN_SPLIT_WIDTHS)[:-1])
    (dn_q, dn_k, dn_v, dn_z, dn_beta, dn_a, fox_q, fox_k, fox_v, fox_f,
     swa_q, swa_k, swa_v, gate_logits) = jnp.split(jnp.einsum('bsd,de->bse', h, w_in), offsets, axis=-1)
    o_a = gated_deltanet(dn_q, dn_k, dn_v, dn_z, dn_beta, dn_a, conv_w, a_log, dt_bias, dn_norm)
    o_b = forgetting_attention(fox_q, fox_k, fox_v, fox_f, b_forget)
    o_c = sliding_window_attention(swa_q, swa_k, swa_v, sinks, cos, sin)
    gates = jax.nn.sigmoid(gate_logits.astype(F32)).astype(h.dtype).reshape(bsz, seq, N_BRANCH, D_MODEL)
    merged = (gates[:, :, 0] * jnp.einsum('bsc,cd->bsd', o_a, w_branch[0])
              + gates[:, :, 1] * jnp.einsum('bsc,cd->bsd', o_b, w_branch[1])
              + gates[:, :, 2] * jnp.einsum('bsc,cd->bsd', o_c, w_branch[2]))
    return jnp.einsum('bsd,de->bse', merged, w_out)


def swiglu(h, w_gate, w_up, w_down):
    return jnp.einsum('bsf,fd->bsd', jax.nn.silu(jnp.einsum('bsd,df->bsf', h, w_gate)) * jnp.einsum('bsd,df->bsf', h, w_up), w_down)


def moe_swiglu(h, w_router, w_gate, w_up, w_down):
    logits = jnp.einsum('bsd,de->bse', h, w_router).astype(F32)
    top_val, top_idx = lax.top_k(logits, TOP_K)
    top_w = jax.nn.softmax(top_val, axis=-1)
    combine = jnp.sum(jax.nn.one_hot(top_idx, N_EXPERTS, dtype=F32) * top_w[..., None], axis=-2).astype(h.dtype)
    out = combine[..., 0:1] * swiglu(h, w_gate[0], w_up[0], w_down[0])
    for e in range(1, N_EXPERTS):
        out = out + combine[..., e:e + 1] * swiglu(h, w_gate[e], w_up[e], w_down[e])
    return out


def setup_inputs(seed: int = 0) -> dict:
    key = jax.random.key(seed)
    ks = jax.random.split(key, 24)

    def nrm(k, shape, scale):
        return jax.random.normal(k, shape, F32) * scale

    x = nrm(ks[0], (BATCH, SEQ, D_MODEL), 1.0)
    c = nrm(ks[1], (BATCH, D_MODEL), 1.0)
    positions = (jax.random.randint(ks[2], (BATCH, 1), 0, MAX_POS_OFFSET, dtype=jnp.int32)
                 + jnp.arange(SEQ, dtype=jnp.int32)[None, :]).astype(jnp.int32)
    w_ada = nrm(ks[3], (DEPTH, D_MODEL, 6 * D_MODEL), 0.5 * D_MODEL ** -0.5)
    b_ada = nrm(ks[4], (DEPTH, 6 * D_MODEL), 0.02)
    norm_mix = 1.0 + nrm(ks[5], (DEPTH, D_MODEL), 0.02)
    w_in = nrm(ks[6], (DEPTH, D_MODEL, IN_WIDTH), D_MODEL ** -0.5)
    conv_w = nrm(ks[7], (DEPTH, DN_CONV, 3 * DN_WIDTH), DN_CONV ** -0.5)
    dn_a_log = jnp.log(jax.random.uniform(ks[8], (DEPTH, DN_HEADS), F32, 1.0, 16.0))
    dt = jnp.exp(jax.random.uniform(ks[9], (DEPTH, DN_HEADS), F32, math.log(1e-3), math.log(1e-1)))
    dn_dt_bias = dt + jnp.log(-jnp.expm1(-dt))
    dn_norm = 1.0 + nrm(ks[10], (DEPTH, DN_HEAD_DIM), 0.02)
    fox_b_forget = jax.random.uniform(ks[11], (DEPTH, FOX_HEADS), F32, 1.0, 4.0)
    swa_sinks = nrm(ks[12], (DEPTH, SWA_Q_HEADS), 1.0)
    w_branch = nrm(ks[13], (DEPTH, N_BRANCH, BRANCH_WIDTH, D_MODEL), BRANCH_WIDTH ** -0.5)
    w_out = nrm(ks[14], (DEPTH, D_MODEL, D_MODEL), D_MODEL ** -0.5)
    norm_ffn = 1.0 + nrm(ks[15], (DEPTH, D_MODEL), 0.02)
    ffn_w_gate = nrm(ks[16], (N_DENSE, D_MODEL, D_FF), D_MODEL ** -0.5)
    ffn_w_up = nrm(ks[17], (N_DENSE, D_MODEL, D_FF), D_MODEL ** -0.5)
    ffn_w_down = nrm(ks[18], (N_DENSE, D_FF, D_MODEL), D_FF ** -0.5)
    moe_router = nrm(ks[19], (N_MOE, D_MODEL, N_EXPERTS), D_MODEL ** -0.5)
    moe_w_gate = nrm(ks[20], (N_MOE, N_EXPERTS, D_MODEL, D_FF), D_MODEL ** -0.5)
    moe_w_up = nrm(ks[21], (N_MOE, N_EXPERTS, D_MODEL, D_FF), D_MODEL ** -0.5)
    moe_w_down = nrm(ks[22], (N_MOE, N_EXPERTS, D_FF, D_MODEL), D_FF ** -0.5)
    final_norm = 1.0 + nrm(ks[23], (D_MODEL,), 0.02)
    return {"x": x, "c": c, "positions": positions, "w_ada": w_ada, "b_ada": b_ada,
            "norm_mix": norm_mix, "w_in": w_in, "conv_w": conv_w, "dn_a_log": dn_a_log,
            "dn_dt_bias": dn_dt_bias, "dn_norm": dn_norm, "fox_b_forget": fox_b_forget,
            "swa_sinks": swa_sinks, "w_branch": w_branch, "w_out": w_out, "norm_ffn": norm_ffn,
            "ffn_w_gate": ffn_w_gate, "ffn_w_up": ffn_w_up, "ffn_w_down": ffn_w_down,
            "moe_router": moe_router, "moe_w_gate": moe_w_gate, "moe_w_up": moe_w_up,
            "moe_w_down": moe_w_down, "final_norm": final_norm}


def reference(x, c, positions, w_ada, b_ada, norm_mix, w_in, conv_w, dn_a_log, dn_dt_bias, dn_norm,
              fox_b_forget, swa_sinks, w_branch, w_out, norm_ffn, ffn_w_gate, ffn_w_up, ffn_w_down,
              moe_router, moe_w_gate, moe_w_up, moe_w_down, final_norm):
    inv_freq = ROPE_THETA ** (-jnp.arange(0, SWA_HEAD_DIM, 2, dtype=F32) / SWA_HEAD_DIM)
    ang = positions.astype(F32)[..., None] * inv_freq
    cos, sin = jnp.cos(ang)[:, :, None, :], jnp.sin(ang)[:, :, None, :]
    c_act = jax.nn.silu(c)
    for layer in range(DEPTH):
        mod = jnp.einsum('bd,de->be', c_act, w_ada[layer]) + b_ada[layer]
        sh1, sc1, g1, sh2, sc2, g2 = jnp.split(mod, 6, axis=-1)
        h = modulate(rms_norm(x, norm_mix[layer]), sh1, sc1)
        x = x + g1[:, None, :] * token_mixer(h, cos, sin, w_in[layer], conv_w[layer], dn_a_log[layer],
                                             dn_dt_bias[layer], dn_norm[layer], fox_b_forget[layer],
                                             swa_sinks[layer], w_branch[layer], w_out[layer])
        h = modulate(rms_norm(x, norm_ffn[layer]), sh2, sc2)
        if layer % 2 == 0:
            i = layer // 2
            f = swiglu(h, ffn_w_gate[i], ffn_w_up[i], ffn_w_down[i])
        else:
            i = layer // 2
            f = moe_swiglu(h, moe_router[i], moe_w_gate[i], moe_w_up[i], moe_w_down[i])
        x = x + g2[:, None, :] * f
    return rms_norm(x, final_norm)
```

```python
import numpy as np
import ml_dtypes
import concourse.bass as bass
import concourse.mybir as mybir
from concourse.bass_utils import run_bass_kernel_spmd

F32 = mybir.dt.float32
BF16 = mybir.dt.bfloat16
I32 = mybir.dt.int32
AF = mybir.ActivationFunctionType
ALU = mybir.AluOpType
AX = mybir.AxisListType

D = 2048
KC = D // 128
SEQ = 4096
NB = 4
DFF = 7168
FC = DFF // 128
NEXP = 8
EPS = 1e-6
IN_W = 14616
GATE_OFF = 8472

SAME_SYNC = True
DN_STOP = 99
DN_X = 3


class Tok:
    __slots__ = ("sem", "val")

    def __init__(self, sem, val=None):
        self.sem = sem
        self.val = val


class Buf:
    __slots__ = ("name", "w", "r", "extra")

    def __init__(self, name):
        self.name = name
        self.w = None
        self.r = {}
        self.extra = []


class Eng:
    def __init__(self, name, sem):
        self.name = name
        self.sem = sem
        self.n = 0
        self.seen = {}
        self.prog = []
        self.pending = []
        self.dsems = []
        self.dcnt = []
        self.dnext = 0


class FW:
    def __init__(self, nc, ndma=6):
        self.nc = nc
        self.E = {}
        for name in ("pe", "act", "dve", "pool", "sp"):
            e = Eng(name, nc.alloc_semaphore("c_" + name))
            self.E[name] = e
        for name in ("sp", "act", "pool"):
            e = self.E[name]
            k = ndma if name != "act" else 2
            e.dsems = [nc.alloc_semaphore(f"d_{name}{i}") for i in range(k)]
            e.dcnt = [0] * k
        self.ps_banks = []
        self.ps_next = 0

    def _waits(self, E, toks):
        need = {}
        for t in toks:
            if t.sem is E.sem:
                if E.name == "pe" or not SAME_SYNC:
                    continue
                if t.val is None:
                    continue
            assert t.val is not None, "wait on unsignaled op"
            k = id(t.sem)
            if E.seen.get(k, 0) >= t.val:
                continue
            if k not in need or need[k][1] < t.val:
                need[k] = (t.sem, t.val)
        for s, v in need.values():
            E.seen[id(s)] = v
        return list(need.values())

    def _deps(self, reads, writes):
        toks = []
        for b in reads:
            if b.w is not None:
                toks.append(b.w)
            toks.extend(b.extra)
        for b in writes:
            if b.w is not None:
                toks.append(b.w)
            toks.extend(b.r.values())
        return toks

    def op(self, en, fn, reads=(), writes=(), sig=True):
        E = self.E[en]
        waits = self._waits(E, self._deps(reads, writes))
        if sig:
            E.n += 1
            tok = Tok(E.sem, E.n)
            for t in E.pending:
                t.val = E.n
            E.pending = []
            inc = (E.sem, 1)
        else:
            tok = Tok(E.sem, None)
            E.pending.append(tok)
            inc = None
        E.prog.append((waits, fn, inc))
        for b in reads:
            b.r[id(E.sem)] = tok
        for b in writes:
            b.w = tok
            b.r = {}
            b.extra = []
        return tok

    def dma(self, en, out, in_, reads=(), writes=(), **kw):
        E = self.E[en]
        i = E.dnext
        E.dnext = (i + 1) % len(E.dsems)
        s = E.dsems[i]
        toks = self._deps(reads, writes)
        if E.dcnt[i] > 0:
            toks.append(Tok(s, E.dcnt[i]))
        waits = self._waits(E, toks)
        E.dcnt[i] += 16
        tok = Tok(s, E.dcnt[i])
        E.prog.append((waits, lambda e: e.dma_start(out=out, in_=in_, **kw), (s, 16)))
        for b in reads:
            b.r[id(s)] = tok
        for b in writes:
            b.w = tok
            b.r = {}
            b.extra = []
        return tok

    def finish(self):
        waits = []
        for E in self.E.values():
            for s, c in zip(E.dsems, E.dcnt):
                if c > 0:
                    waits.append((s, c))
        self.E["sp"].prog.append((waits, None, None))

    def emit(self):
        nc = self.nc

        def mk(E):
            def body(eng):
                for waits, fn, inc in E.prog:
                    for s, v in waits:
                        eng.wait_ge(s, v)
                    if fn is None:
                        continue
                    ins = fn(eng)
                    if inc is not None:
                        ins.then_inc(inc[0], inc[1])
            return body

        with nc.Block() as block:
            block.tensor(mk(self.E["pe"]))
            block.vector(mk(self.E["dve"]))
            block.scalar(mk(self.E["act"]))
            block.gpsimd(mk(self.E["pool"]))
            block.sync(mk(self.E["sp"]))

    def init_psum(self):
        for i in range(8):
            t = self.nc.alloc_psum_tensor(f"psb{i}", [128, 512], F32)
            self.ps_banks.append((t, Buf(f"psb{i}")))

    def psum(self):
        self.ps_next = self.ps_next % len(self.ps_banks)
        t, b = self.ps_banks[self.ps_next]
        self.ps_next = (self.ps_next + 1) % len(self.ps_banks)
        return t, b

    def psum_reserve(self, n):
        out = [self.ps_banks.pop() for _ in range(n)]
        return out

    def psum_release(self, banks):
        self.ps_banks.extend(banks)


class Ring:
    def __init__(self, nc, name, n, shape, dtype):
        self.t = [nc.alloc_sbuf_tensor(f"{name}{i}", shape, dtype) for i in range(n)]
        self.b = [Buf(f"{name}{i}") for i in range(n)]
        self.i = 0
        self.n = n

    def next(self):
        k = self.i
        self.i = (k + 1) % self.n
        return self.t[k], self.b[k]


class WStream:
    def __init__(self, fw, ring, tiles, look, queue="sp"):
        self.fw, self.ring, self.tiles, self.look, self.queue = fw, ring, tiles, look, queue
        self.issued = 0
        self.got = 0
        self.slots = {}

    def _issue(self):
        k = self.issued
        src, n, sbuf_ = self.tiles[k]
        t, b = self.ring.next()
        self.fw.dma(self.queue, t[:, 0:n], src, reads=[sbuf_], writes=[b])
        self.slots[k] = (t, b)
        self.issued += 1

    def get(self):
        while self.issued < len(self.tiles) and self.issued <= self.got + self.look:
            self._issue()
        t, b = self.slots.pop(self.got)
        self.got += 1
        return t, b


def bcast_ap(ap, shape):
    return ap.to_broadcast(shape)


TT = 512
WBUF = 8192


def build_B(T=2048, moe=False, final=False, do_mix=True, do_ffn=True):
    nc = bass.Bass("TRN2", target_bir_lowering=False)
    fw = FW(nc)
    fw.init_psum()
    NT = T // TT
    NE = NEXP if moe else 1

    def din(name, shape, dt=F32):
        return nc.dram_tensor(name, shape, dt, kind="ExternalInput").ap()

    xT = din("xT", [D, T])
    vecs = din("vecs", [128, 9 * KC])
    yT = nc.dram_tensor("yT", [D, T], F32, kind="ExternalOutput").ap()
    if do_mix:
        oT = din("oT", [3 * 1024, T], BF16)
        w_gate = din("w_gate", [D, 3 * D])
        w_branch = din("w_branch", [3 * 1024, D])
        w_out = din("w_out", [D, D])
    if do_ffn:
        wg = din("wg", [NE, D, DFF])
        wu = din("wu", [NE, D, DFF])
        wd = din("wd", [NE, DFF, D])
        if moe:
            w_router = din("w_router", [D, NEXP])

    def dscr(name, shape):
        return nc.dram_tensor(name, shape, BF16).ap()

    pre = []
    sbufs = {}

    def skey(k):
        if k not in sbufs:
            sbufs[k] = Buf("scr" + str(k))
        return sbufs[k]
    if do_mix:
        WG_s = dscr("WG_s", [16, 128, KC * 3 * 128])
        WB_s = dscr("WB_s", [16, 128, 24 * 128])
        WO_s = dscr("WO_s", [4, 128, KC * 512])
        wgv = w_gate.rearrange("(kc p) c -> p kc c", p=128)
        wbv = w_branch.rearrange("(cc p) c -> p cc c", p=128)
        wov = w_out.rearrange("(kc p) c -> p kc c", p=128)
        for dc in range(16):
            for r in range(3):
                dst = WG_s[dc].rearrange("p (kc r c) -> p kc r c", kc=KC, r=3)[:, :, r, :]
                pre.append((dst, wgv[:, :, r * D + dc * 128: r * D + dc * 128 + 128], ('g', dc)))
            pre.append((WB_s[dc].rearrange("p (cc c) -> p cc c", c=128), wbv[:, :, dc * 128:(dc + 1) * 128], ('b', dc)))
        for eg in range(4):
            pre.append((WO_s[eg].rearrange("p (kc c) -> p kc c", c=512), wov[:, :, eg * 512:(eg + 1) * 512], ('o', eg)))
    if do_ffn:
        WF_s = [dscr(f"WF_s{e}", [28, 128, KC * 2 * 256]) for e in range(NE)]
        WD_s = [dscr(f"WD_s{e}", [16, 128, FC * 128]) for e in range(NE)]
        for e in range(NE):
            wgv2 = wg[e].rearrange("(kc p) c -> p kc c", p=128)
            wuv2 = wu[e].rearrange("(kc p) c -> p kc c", p=128)
            wdv2 = wd[e].rearrange("(fc p) c -> p fc c", p=128)
            for fg in range(28):
                dstv = WF_s[e][fg].rearrange("p (kc g c) -> p kc g c", kc=KC, g=2)
                pre.append((dstv[:, :, 0, :], wgv2[:, :, fg * 256:(fg + 1) * 256], ('f', e, fg)))
                pre.append((dstv[:, :, 1, :], wuv2[:, :, fg * 256:(fg + 1) * 256], ('f', e, fg)))
            for eg in range(16):
                for hf in range(2):
                    dstv = WD_s[e][eg].rearrange("p (fc c) -> p fc c", c=128)
                    pre.append((dstv[:, hf * 28:(hf + 1) * 28, :], wdv2[:, hf * 28:(hf + 1) * 28, eg * 128:(eg + 1) * 128], ('d', e, eg)))
    for dst, src, k in pre:
        b_ = skey(k)
        w_old = b_.w
        tk = fw.dma("pool", dst, src, writes=[])
        b_.r = {}
        if w_old is None:
            b_.w = tk
            b_.extra = []
        else:
            b_.extra.append(tk)

    sb = nc.alloc_sbuf_tensor
    xt = sb("xt", [128, KC, TT], F32); xt_b = Buf("xt")
    ht = sb("ht", [128, KC, TT], BF16); ht_b = Buf("ht")
    vec_t = sb("vec_t", [128, 9 * KC], F32); vec_b = Buf("vec")
    AB = sb("AB", [128, 4 * KC], F32); AB_b = Buf("AB")
    ones_bf = sb("ones_bf", [128, 128], BF16); ones_b = Buf("ones")
    sq = Ring(nc, "sq", 2, [128, TT], BF16)
    rstd = sb("rstd", [128, TT], F32); rstd_b = Buf("rstd")
    tmp = Ring(nc, "tmp", 3, [128, TT], F32)
    wring = Ring(nc, "wr", 4, [128, WBUF], BF16)
    at = sb("at", [128, FC, TT], BF16); at_b = Buf("at")
    if do_mix:
        ot = at[:, 0:24, :]; ot_b = Buf("ot")
        mt = at[:, 24:40, :]; mt_b = Buf("mt")
        macc = sb("macc", [128, TT], F32); macc_b = Buf("macc")
    if do_ffn:
        sgr = Ring(nc, "sg", 2, [128, TT], BF16)
    if moe:
        h32 = at[:].rearrange("p a b -> p (a b)")[:, 0:2 * KC * TT].bitcast(F32).rearrange("p (k t) -> p k t", t=TT); h32_b = Buf("h32")
        wr_t = sb("wr_t", [128, KC, NEXP], F32); wr_b = Buf("wr_t")
        ident = sb("ident", [128, 128], F32); ident_b = Buf("ident")
        lgT = sb("lgT", [NEXP, TT], F32); lgT_b = Buf("lgT")
        lg = sb("lg", [128, 4, NEXP], F32); lg_b = Buf("lg")
        mx8 = sb("mx8", [128, 4, 8], F32); mx8_b = Buf("mx8")
        cmb = sb("cmb", [128, 4, NEXP], F32); cmb_b = Buf("cmb")
        cmbT = sb("cmbT", [NEXP, TT], F32); cmbT_b = Buf("cmbT")
        sel = sb("sel", [NEXP, NEXP * 128], F32); sel_b = Buf("sel")
        cb = sb("cb", [128, TT], F32); cb_b = Buf("cb")
        sm1 = sb("sm1", [128, 4, 4], F32); sm1_b = Buf("sm1")

    vsl = lambda j: vec_t[:, j * KC:(j + 1) * KC]

    fw.dma("sp", vec_t[:], vecs, writes=[vec_b])
    fw.op("pool", lambda e: e.memset(ones_bf[:], 1.0), writes=[ones_b])
    fw.op("dve", lambda e: e.scalar_tensor_tensor(out=AB[:, 0:KC], in0=vsl(1), scalar=1.0, in1=vsl(6), op0=ALU.add, op1=ALU.mult), reads=[vec_b], writes=[AB_b])
    fw.op("dve", lambda e: e.tensor_copy(out=AB[:, KC:2 * KC], in_=vsl(0)), reads=[vec_b], writes=[AB_b])
    fw.op("dve", lambda e: e.scalar_tensor_tensor(out=AB[:, 2 * KC:3 * KC], in0=vsl(4), scalar=1.0, in1=vsl(7), op0=ALU.add, op1=ALU.mult), reads=[vec_b], writes=[AB_b])
    fw.op("dve", lambda e: e.tensor_copy(out=AB[:, 3 * KC:4 * KC], in_=vsl(3)), reads=[vec_b], writes=[AB_b])
    if moe:
        fw.dma("sp", wr_t[:], w_router.rearrange("(kc p) e -> p kc e", p=128), writes=[wr_b])
        fw.dma("sp", ident[:], din("c_ident", [128, 128]), writes=[ident_b])
        fw.dma("sp", sel[:], din("c_sel", [NEXP, NEXP * 128]), writes=[sel_b])

    def norm(Acol, Bcol, want32=False):
        pt, pb = fw.psum()
        for kc in range(KC):
            st, sbf = sq.next()
            fw.op("act", lambda e, kc=kc, st=st: e.activation(out=st[:], in_=xt[:, kc, :], func=AF.Square), reads=[xt_b], writes=[sbf])
            fw.op("pe", lambda e, kc=kc, st=st: e.matmul(pt[:], ones_bf[:], st[:], start=(kc == 0), stop=(kc == KC - 1)), reads=[sbf, ones_b], writes=[pb], sig=True)
        fw.op("act", lambda e: e.activation(out=rstd[:], in_=pt[:], func=AF.Sqrt, scale=1.0 / D, bias=EPS), reads=[pb], writes=[rstd_b])
        fw.op("dve", lambda e: e.reciprocal(out=rstd[:], in_=rstd[:]), reads=[rstd_b], writes=[rstd_b])
        for kc in range(KC):
            tt_, tb_ = tmp.next()
            fw.op("dve", lambda e, kc=kc, tt_=tt_: e.scalar_tensor_tensor(out=tt_[:], in0=xt[:, kc, :], scalar=AB[:, Acol + kc:Acol + kc + 1], in1=rstd[:], op0=ALU.mult, op1=ALU.mult), reads=[xt_b, AB_b, rstd_b], writes=[tb_])
            if want32:
                fw.op("act", lambda e, kc=kc, tt_=tt_: e.activation(out=h32[:, kc, :], in_=tt_[:], func=AF.Identity, bias=AB[:, Bcol + kc:Bcol + kc + 1], scale=1.0), reads=[tb_, AB_b], writes=[h32_b, at_b] + ([ot_b, mt_b] if do_mix else []))
                fw.op("pool", lambda e, kc=kc: e.tensor_copy(out=ht[:, kc, :], in_=h32[:, kc, :]), reads=[h32_b], writes=[ht_b])
            else:
                fw.op("act", lambda e, kc=kc, tt_=tt_: e.activation(out=ht[:, kc, :], in_=tt_[:], func=AF.Identity, bias=AB[:, Bcol + kc:Bcol + kc + 1], scale=1.0), reads=[tb_, AB_b], writes=[ht_b])

    def wtiles():
        L = []
        if do_mix:
            for dc in range(16):
                L.append((WG_s[dc], KC * 384, skey(('g', dc))))
                L.append((WB_s[dc], 24 * 128, skey(('b', dc))))
            for eg in range(4):
                L.append((WO_s[eg], KC * 512, skey(('o', eg))))
        if do_ffn:
            for e in range(NE):
                for fg in range(28):
                    L.append((WF_s[e][fg], KC * 512, skey(('f', e, fg))))
                for eg in range(16):
                    L.append((WD_s[e][eg], FC * 128, skey(('d', e, eg))))
        return L

    tiles = []
    for _ in range(NT):
        tiles += wtiles()
    ws = WStream(fw, wring, tiles, look=2)

    xTv = xT.rearrange("(kc p) t -> p kc t", p=128)
    yTv = yT.rearrange("(kc p) t -> p kc t", p=128)
    if do_mix:
        oTv = oT.rearrange("(cc p) t -> p cc t", p=128)

    for ti in range(NT):
        t0 = ti * TT
        for q4 in range(4):
            fw.dma("sp", xt[:, q4 * 4:(q4 + 1) * 4, :], xTv[:, q4 * 4:(q4 + 1) * 4, t0:t0 + TT], writes=[xt_b])
        if do_mix:
            for q4 in range(3):
                fw.dma("sp", ot[:, q4 * 8:(q4 + 1) * 8, :], oTv[:, q4 * 8:(q4 + 1) * 8, t0:t0 + TT], writes=[ot_b, at_b])
            norm(0, KC)
            for dc in range(16):
                wgt, wgb = ws.get()
                wbt, wbb = ws.get()
                wgv_ = wgt[:, 0:KC * 384].rearrange("p (kc r c) -> p kc r c", kc=KC, r=3)
                wbv_ = wbt[:, 0:24 * 128].rearrange("p (cc c) -> p cc c", c=128)
                for r in range(3):
                    gt, gb = fw.psum()
                    bt, bb = fw.psum()
                    for kc in range(KC):
                        fw.op("pe", lambda e, kc=kc, r=r, gt=gt, wgv_=wgv_: e.matmul(gt[:], wgv_[:, kc, r, :], ht[:, kc, :], start=(kc == 0), stop=(kc == KC - 1)), reads=[wgb, ht_b], writes=[gb], sig=(kc == KC - 1))
                    for cc in range(8):
                        fw.op("pe", lambda e, cc=cc, r=r, bt=bt, wbv_=wbv_: e.matmul(bt[:], wbv_[:, r * 8 + cc, :], ot[:, r * 8 + cc, :], start=(cc == 0), stop=(cc == 7)), reads=[wbb, ot_b], writes=[bb], sig=(cc == 7))
                    st_, sb_ = tmp.next()
                    fw.op("act", lambda e, gt=gt, st_=st_: e.activation(out=st_[:], in_=gt[:], func=AF.Sigmoid), reads=[gb], writes=[sb_])
                    if r == 0:
                        fw.op("dve", lambda e, bt=bt, st_=st_: e.tensor_tensor(out=macc[:], in0=bt[:], in1=st_[:], op=ALU.mult), reads=[bb, sb_], writes=[macc_b])
                    else:
                        fw.op("dve", lambda e, bt=bt, st_=st_: e.tensor_tensor(out=st_[:], in0=bt[:], in1=st_[:], op=ALU.mult), reads=[bb, sb_], writes=[sb_])
                        if r == 1:
                            fw.op("pool", lambda e, st_=st_: e.tensor_tensor(out=macc[:], in0=macc[:], in1=st_[:], op=ALU.add), reads=[sb_, macc_b], writes=[macc_b])
                        else:
                            fw.op("pool", lambda e, st_=st_, dc=dc: e.tensor_tensor(out=mt[:, dc, :], in0=macc[:], in1=st_[:], op=ALU.add), reads=[sb_, macc_b], writes=[mt_b])
            for eg in range(4):
                wot, wob = ws.get()
                wov_ = wot[:, 0:KC * 512].rearrange("p (kc c) -> p kc c", c=512)
                for ec in range(4):
                    e_ = eg * 4 + ec
                    pt, pb = fw.psum()
                    for kc in range(KC):
                        fw.op("pe", lambda e, kc=kc, ec=ec, pt=pt, wov_=wov_: e.matmul(pt[:], wov_[:, kc, ec * 128:(ec + 1) * 128], mt[:, kc, :], start=(kc == 0), stop=(kc == KC - 1)), reads=[wob, mt_b], writes=[pb], sig=(kc == KC - 1))
                    fw.op("dve", lambda e, e_=e_, pt=pt: e.scalar_tensor_tensor(out=xt[:, e_, :], in0=pt[:], scalar=vec_t[:, 2 * KC + e_:2 * KC + e_ + 1], in1=xt[:, e_, :], op0=ALU.mult, op1=ALU.add), reads=[pb, vec_b, xt_b], writes=[xt_b])
        if do_ffn:
            norm(2 * KC, 3 * KC, want32=moe)
            if moe:
                pt, pb = fw.psum()
                for kc in range(KC):
                    fw.op("pe", lambda e, kc=kc, pt=pt: e.matmul(pt[0:NEXP, :], wr_t[:, kc, :], h32[:, kc, :], start=(kc == 0), stop=(kc == KC - 1)), reads=[wr_b, h32_b], writes=[pb], sig=(kc == KC - 1))
                fw.op("act", lambda e, pt=pt: e.copy(out=lgT[:], in_=pt[0:NEXP, :]), reads=[pb], writes=[lgT_b])
                pt2, pb2 = fw.psum()
                for j in range(4):
                    fw.op("pe", lambda e, j=j, pt2=pt2: e.matmul(pt2[:, j * NEXP:(j + 1) * NEXP], lgT[:, j * 128:(j + 1) * 128], ident[0:NEXP, 0:NEXP], start=True, stop=True), reads=[lgT_b, ident_b], writes=[pb2], sig=(j == 3))
                fw.op("dve", lambda e, pt2=pt2: e.tensor_copy(out=lg[:].rearrange("p a b -> p (a b)"), in_=pt2[:, 0:4 * NEXP]), reads=[pb2], writes=[lg_b])
                for j in range(4):
                    fw.op("dve", lambda e, j=j: e.max(out=mx8[:, j, :], in_=lg[:, j, :]), reads=[lg_b], writes=[mx8_b])
                fw.op("dve", lambda e: e.tensor_tensor(out=cmb[:], in0=lg[:], in1=mx8[:, :, 0:1].to_broadcast([128, 4, NEXP]), op=ALU.subtract), reads=[lg_b, mx8_b], writes=[cmb_b])
                fw.op("act", lambda e: e.activation(out=cmb[:], in_=cmb[:], func=AF.Exp), reads=[cmb_b], writes=[cmb_b])
                fw.op("dve", lambda e: e.tensor_tensor(out=sm1[:, :, 0:1], in0=mx8[:, :, 1:2], in1=mx8[:, :, 0:1], op=ALU.subtract), reads=[mx8_b], writes=[sm1_b])
                fw.op("act", lambda e: e.activation(out=sm1[:, :, 1:2], in_=sm1[:, :, 0:1], func=AF.Exp), reads=[sm1_b], writes=[sm1_b])
                fw.op("dve", lambda e: e.tensor_scalar(out=sm1[:, :, 2:3], in0=sm1[:, :, 1:2], scalar1=1.0, scalar2=None, op0=ALU.add), reads=[sm1_b], writes=[sm1_b])
                fw.op("dve", lambda e: e.reciprocal(out=sm1[:, :, 3:4], in_=sm1[:, :, 2:3]), reads=[sm1_b], writes=[sm1_b])
                fw.op("dve", lambda e: e.tensor_tensor(out=cmb[:], in0=cmb[:], in1=sm1[:, :, 3:4].to_broadcast([128, 4, NEXP]), op=ALU.mult), reads=[cmb_b, sm1_b], writes=[cmb_b])
                fw.op("dve", lambda e: e.tensor_tensor(out=lg[:], in0=lg[:], in1=mx8[:, :, 1:2].to_broadcast([128, 4, NEXP]), op=ALU.is_ge), reads=[lg_b, mx8_b], writes=[lg_b])
                fw.op("dve", lambda e: e.tensor_tensor(out=cmb[:], in0=cmb[:], in1=lg[:], op=ALU.mult), reads=[cmb_b, lg_b], writes=[cmb_b])
                pt3, pb3 = fw.psum()
                for j in range(4):
                    fw.op("pe", lambda e, j=j, pt3=pt3: e.matmul(pt3[0:NEXP, j * 128:(j + 1) * 128], cmb[:, j, :], ident[:, :], start=True, stop=True), reads=[cmb_b, ident_b], writes=[pb3], sig=(j == 3))
                fw.op("act", lambda e, pt3=pt3: e.copy(out=cmbT[:], in_=pt3[0:NEXP, :]), reads=[pb3], writes=[cmbT_b])
            for ex in range(NE):
                if moe:
                    ptc, pbc = fw.psum()
                    fw.op("pe", lambda e, ex=ex, ptc=ptc: e.matmul(ptc[:], sel[:, ex * 128:(ex + 1) * 128], cmbT[:], start=True, stop=True), reads=[sel_b, cmbT_b], writes=[pbc])
                    fw.op("act", lambda e, ptc=ptc: e.copy(out=cb[:], in_=ptc[:]), reads=[pbc], writes=[cb_b])
                for fg in range(28):
                    wft, wfb = ws.get()
                    wfv_ = wft[:, 0:KC * 512].rearrange("p (kc g c) -> p kc g c", kc=KC, g=2)
                    for fc2 in range(2):
                        f_ = fg * 2 + fc2
                        gt, gb = fw.psum()
                        ut, ub = fw.psum()
                        for kc in range(KC):
                            fw.op("pe", lambda e, kc=kc, fc2=fc2, gt=gt, wfv_=wfv_: e.matmul(gt[:], wfv_[:, kc, 0, fc2 * 128:(fc2 + 1) * 128], ht[:, kc, :], start=(kc == 0), stop=(kc == KC - 1)), reads=[wfb, ht_b], writes=[gb], sig=(kc == KC - 1))
                        for kc in range(KC):
                            fw.op("pe", lambda e, kc=kc, fc2=fc2, ut=ut, wfv_=wfv_: e.matmul(ut[:], wfv_[:, kc, 1, fc2 * 128:(fc2 + 1) * 128], ht[:, kc, :], start=(kc == 0), stop=(kc == KC - 1)), reads=[wfb, ht_b], writes=[ub], sig=(kc == KC - 1))
                        sg_, sgb_ = sgr.next()
                        fw.op("act", lambda e, gt=gt, sg_=sg_: e.activation(out=sg_[:], in_=gt[:], func=AF.Silu), reads=[gb], writes=[sgb_])
                        fw.op("dve", lambda e, ut=ut, sg_=sg_, f_=f_: e.tensor_tensor(out=at[:, f_, :], in0=ut[:], in1=sg_[:], op=ALU.mult), reads=[ub, sgb_], writes=[at_b])
                for eg in range(16):
                    wdt, wdb = ws.get()
                    wdv_ = wdt[:, 0:FC * 128].rearrange("p (fc c) -> p fc c", c=128)
                    pt, pb = fw.psum()
                    for f_ in range(FC):
                        fw.op("pe", lambda e, f_=f_, pt=pt, wdv_=wdv_: e.matmul(pt[:], wdv_[:, f_, :], at[:, f_, :], start=(f_ == 0), stop=(f_ == FC - 1)), reads=[wdb, at_b], writes=[pb], sig=(f_ == FC - 1))
                    if moe:
                        t2, tb2 = tmp.next()
                        fw.op("dve", lambda e, pt=pt, t2=t2: e.tensor_tensor(out=t2[:], in0=pt[:], in1=cb[:], op=ALU.mult), reads=[pb, cb_b], writes=[tb2])
                        fw.op("dve", lambda e, eg=eg, t2=t2: e.scalar_tensor_tensor(out=xt[:, eg, :], in0=t2[:], scalar=vec_t[:, 5 * KC + eg:5 * KC + eg + 1], in1=xt[:, eg, :], op0=ALU.mult, op1=ALU.add), reads=[tb2, vec_b, xt_b], writes=[xt_b])
                    else:
                        fw.op("dve", lambda e, eg=eg, pt=pt: e.scalar_tensor_tensor(out=xt[:, eg, :], in0=pt[:], scalar=vec_t[:, 5 * KC + eg:5 * KC + eg + 1], in1=xt[:, eg, :], op0=ALU.mult, op1=ALU.add), reads=[pb, vec_b, xt_b], writes=[xt_b])
        if final:
            pt, pb = fw.psum()
            for kc in range(KC):
                st, sbf = sq.next()
                fw.op("act", lambda e, kc=kc, st=st: e.activation(out=st[:], in_=xt[:, kc, :], func=AF.Square), reads=[xt_b], writes=[sbf])
                fw.op("pe", lambda e, kc=kc, st=st, pt=pt: e.matmul(pt[:], ones_bf[:], st[:], start=(kc == 0), stop=(kc == KC - 1)), reads=[sbf, ones_b], writes=[pb], sig=True)
            fw.op("act", lambda e, pt=pt: e.activation(out=rstd[:], in_=pt[:], func=AF.Sqrt, scale=1.0 / D, bias=EPS), reads=[pb], writes=[rstd_b])
            fw.op("dve", lambda e: e.reciprocal(out=rstd[:], in_=rstd[:]), reads=[rstd_b], writes=[rstd_b])
            for kc in range(KC):
                fw.op("dve", lambda e, kc=kc: e.scalar_tensor_tensor(out=xt[:, kc, :], in0=xt[:, kc, :], scalar=vec_t[:, 8 * KC + kc:8 * KC + kc + 1], in1=rstd[:], op0=ALU.mult, op1=ALU.mult), reads=[xt_b, vec_b, rstd_b], writes=[xt_b])
        for q4 in range(4):
            fw.dma("sp", yTv[:, q4 * 4:(q4 + 1) * 4, t0:t0 + TT], xt[:, q4 * 4:(q4 + 1) * 4, :], reads=[xt_b])
    fw.finish()
    fw.emit()
    return nc


class Arena:
    def __init__(self, nc, nbytes, name="arena"):
        self.t = nc.alloc_sbuf_tensor(name, [128, nbytes // 4], F32)
        self.off = 0
        self.cap = nbytes

    def reset(self):
        self.off = 0

    def alloc(self, shape, dtype, parts=128):
        n = 1
        for d_ in shape:
            n *= d_
        sz = 2 if dtype == BF16 else 4
        nb = (n * sz + 63) // 64 * 64
        assert self.off + nb <= self.cap, ("arena overflow", self.off, nb, self.cap)
        a = self.t[0:parts, self.off // 4:(self.off + nb) // 4]
        self.off += nb
        if dtype != F32:
            a = a.bitcast(dtype)
        a = a[:, 0:n]
        if len(shape) == 2:
            a = a.rearrange("p (a b) -> p a b", a=shape[0])
        elif len(shape) == 3:
            a = a.rearrange("p (a b c) -> p a b c", a=shape[0], b=shape[1])
        elif len(shape) == 4:
            a = a.rearrange("p (a b c d) -> p a b c d", a=shape[0], b=shape[1], c=shape[2])
        return a


def fw_barrier(fw):
    for E in fw.E.values():
        toks = []
        for E2 in fw.E.values():
            if E2 is not E and E2.n > 0:
                toks.append(Tok(E2.sem, E2.n))
            for s, c in zip(E2.dsems, E2.dcnt):
                if c > 0:
                    toks.append(Tok(s, c))
        E.prog.append((fw._waits(E, toks), None, None))


def dma_add(fw, en, out, in_, reads=(), wadd=()):
    tk = fw.dma(en, out, in_, reads=reads, writes=[])
    for b in wadd:
        if b.w is None:
            b.w = tk
        else:
            b.extra.append(tk)
    return tk


NCA = 9
CF_ID, CF_U, CF_ONES, CF_INVF, CF_SL = 0, 128, 256, 384, 416
CF_NEG1 = 544
CF_LM = 672
CF_N = 672 + 7 * 128
CB_ID, CB_MC, CB_MP, CB_ONES = 0, 128, 256, 384
CB_N = 512


def build_A(S=4096, parts=("fox", "swa", "dn")):
    nc = bass.Bass("TRN2", target_bir_lowering=False)
    fw = FW(nc)
    fw.init_psum()
    NT = S // TT
    NBLK = S // 128

    def din(name, shape, dt=F32):
        return nc.dram_tensor(name, shape, dt, kind="ExternalInput").ap()

    xT = din("xT", [D, S])
    vecs = din("vecs", [128, 9 * KC])
    wA = din("wA", [D, NCA * 512])
    cf = din("cf", [128, CF_N])
    cbf = din("cbf", [128, CB_N], BF16)
    pos = din("pos", [128, NBLK], I32)
    smallp = din("smallp", [128, 64])
    dnw = din("dnw", [128, 12 * 4 + 128])
    oT = nc.dram_tensor("oT", [1536, S], BF16, kind="ExternalOutput").ap()

    QKT = nc.dram_tensor("QKT", [20, 128, S], BF16).ap()
    TOKB = nc.dram_tensor("TOKB", [S, 1664], BF16).ap()
    TOKS = nc.dram_tensor("TOKS", [S, 16], F32).ap()
    WA_s = nc.dram_tensor("WA_s", [NCA, 128, KC * 512], BF16).ap()
    qkt_b = [Buf(f"qkt{i}") for i in range(20)]
    tokb_b = Buf("tokb")
    toks_b = Buf("toks")
    was_b = [Buf(f"was{i}") for i in range(NCA)]

    wAv = wA.rearrange("(kc p) c -> p kc c", p=128)
    for i in range(NCA):
        for hf in range(2):
            dst = WA_s[i].rearrange("p (kc c) -> p kc c", c=512)[:, hf * 8:(hf + 1) * 8, :]
            dma_add(fw, "pool", dst, wAv[:, hf * 8:(hf + 1) * 8, i * 512:(i + 1) * 512], wadd=[was_b[i]])

    ar = Arena(nc, 172 * 1024)
    sb = nc.alloc_sbuf_tensor
    vec_t = sb("vec_t", [128, 9 * KC], F32); vec_b = Buf("vec")
    AB = sb("AB", [128, 2 * KC], F32); AB_b = Buf("AB")
    cf_t = sb("cf_t", [128, CF_N], F32); cf_b = Buf("cf")
    cb_t = sb("cb_t", [128, CB_N], BF16); cb_b = Buf("cb")
    sm_t = sb("sm_t", [128, 64], F32); sm_b = Buf("sm")
    dnw_t = sb("dnw_t", [128, 12 * 4 + 128], F32); dnw_b = Buf("dnw")
    stg = Ring(nc, "stg", 4, [128, 512], BF16)
    stg32 = Ring(nc, "stg32", 2, [128, 16], F32)
    fw.dma("sp", vec_t[:], vecs, writes=[vec_b])
    fw.dma("sp", cf_t[:], cf, writes=[cf_b])
    fw.dma("sp", cb_t[:], cbf, writes=[cb_b])
    fw.dma("sp", sm_t[:], smallp, writes=[sm_b])
    fw.dma("sp", dnw_t[:], dnw, writes=[dnw_b])
    ident_bf = cb_t[:, CB_ID:CB_ID + 128]
    ones_bf = cb_t[:, CB_ONES:CB_ONES + 128]
    mask_cur = cb_t[:, CB_MC:CB_MC + 128]
    mask_prev = cb_t[:, CB_MP:CB_MP + 128]
    ident_f = cf_t[:, CF_ID:CF_ID + 128]
    U_f = cf_t[:, CF_U:CF_U + 128]
    ones_f = cf_t[:, CF_ONES:CF_ONES + 128]
    vsl = lambda j: vec_t[:, j * KC:(j + 1) * KC]
    fw.op("dve", lambda e: e.scalar_tensor_tensor(out=AB[:, 0:KC], in0=vsl(1), scalar=1.0, in1=vsl(6), op0=ALU.add, op1=ALU.mult), reads=[vec_b], writes=[AB_b])
    fw.op("dve", lambda e: e.tensor_copy(out=AB[:, KC:2 * KC], in_=vsl(0)), reads=[vec_b], writes=[AB_b])

    xt = ar.alloc([KC, TT], F32); xt_b = Buf("xt")
    ht = ar.alloc([KC, TT], BF16); ht_b = Buf("ht")
    rstd = ar.alloc([TT], F32); rstd_b = Buf("rstd")
    sqt = [ar.alloc([TT], BF16) for _ in range(2)]; sq_b = [Buf("sq0"), Buf("sq1")]
    tmpt = [ar.alloc([TT], F32) for _ in range(3)]; tmp_b = [Buf(f"tmp{i}") for i in range(3)]
    wrt = [ar.alloc([WBUF], BF16) for _ in range(4)]; wr_b = [Buf(f"wr{i}") for i in range(4)]

    class _R:
        def __init__(s_, t, b):
            s_.t, s_.b, s_.i = t, b, 0

        def next(s_):
            k = s_.i
            s_.i = (k + 1) % len(s_.t)
            return s_.t[k], s_.b[k]
    sq = _R(sqt, sq_b); tmp = _R(tmpt, tmp_b); wring = _R(wrt, wr_b)

    tiles = []
    for _ in range(NT):
        for i in range(NCA):
            tiles.append((WA_s[i], KC * 512, was_b[i]))
    ws = WStream(fw, wring, tiles, look=2)
    xTv = xT.rearrange("(kc p) t -> p kc t", p=128)
    evq = [0]

    def evac(dst, src, rd, wr):
        evq[0] ^= 1
        if evq[0]:
            fw.op("act", lambda e: e.copy(out=dst, in_=src), reads=rd, writes=wr)
        else:
            fw.op("dve", lambda e: e.tensor_copy(out=dst, in_=src), reads=rd, writes=wr)

    for ti in range(NT):
        t0 = ti * TT
        for q4 in range(4):
            fw.dma("sp", xt[:, q4 * 4:(q4 + 1) * 4, :], xTv[:, q4 * 4:(q4 + 1) * 4, t0:t0 + TT], writes=[xt_b])
        pt, pb = fw.psum()
        for kc in range(KC):
            st, sbf = sq.next()
            fw.op("act", lambda e, kc=kc, st=st: e.activation(out=st, in_=xt[:, kc, :], func=AF.Square), reads=[xt_b], writes=[sbf])
            fw.op("pe", lambda e, kc=kc, st=st, pt=pt: e.matmul(pt[:], ones_bf, st, start=(kc == 0), stop=(kc == KC - 1)), reads=[sbf, cb_b], writes=[pb], sig=True)
        fw.op("act", lambda e, pt=pt: e.activation(out=rstd, in_=pt[:], func=AF.Sqrt, scale=1.0 / D, bias=EPS), reads=[pb], writes=[rstd_b])
        fw.op("dve", lambda e: e.reciprocal(out=rstd, in_=rstd), reads=[rstd_b], writes=[rstd_b])
        for kc in range(KC):
            tt_, tb_ = tmp.next()
            fw.op("dve", lambda e, kc=kc, tt_=tt_: e.scalar_tensor_tensor(out=tt_, in0=xt[:, kc, :], scalar=AB[:, kc:kc + 1], in1=rstd, op0=ALU.mult, op1=ALU.mult), reads=[xt_b, AB_b, rstd_b], writes=[tb_])
            fw.op("act", lambda e, kc=kc, tt_=tt_: e.activation(out=ht[:, kc, :], in_=tt_, func=AF.Identity, bias=AB[:, KC + kc:KC + kc + 1], scale=1.0), reads=[tb_, AB_b], writes=[ht_b])
        for wi in range(5):
            wt, wb = ws.get()
            wv = wt[:, 0:KC * 512].rearrange("p (kc c) -> p kc c", c=512)
            for c4 in range(4):
                ch = wi * 4 + c4
                pt, pb = fw.psum()
                for kc in range(KC):
                    fw.op("pe", lambda e, kc=kc, c4=c4, pt=pt, wv=wv: e.matmul(pt[:], wv[:, kc, c4 * 128:(c4 + 1) * 128], ht[:, kc, :], start=(kc == 0), stop=(kc == KC - 1)), reads=[wb, ht_b], writes=[pb], sig=(kc == KC - 1))
                sg, sgb = stg.next()
                evac(sg[:], pt[:], [pb], [sgb])
                dma_add(fw, "sp", QKT[ch][:, t0:t0 + TT], sg[:], reads=[sgb], wadd=[qkt_b[ch]])
        for gi in range(4):
            wt, wb = ws.get()
            wv = wt[:, 0:KC * 512].rearrange("p (kc c) -> p kc c", c=512)
            ncol = 512 if gi < 3 else 140
            for blk in range(4):
                pt, pb = fw.psum()
                for kc in range(KC):
                    fw.op("pe", lambda e, kc=kc, blk=blk, pt=pt, wv=wv, ncol=ncol: e.matmul(pt[:, 0:ncol], ht[:, kc, blk * 128:(blk + 1) * 128], wv[:, kc, 0:ncol], start=(kc == 0), stop=(kc == KC - 1)), reads=[wb, ht_b], writes=[pb], sig=(kc == KC - 1))
                sg, sgb = stg.next()
                r0 = t0 + blk * 128
                if gi < 3:
                    evac(sg[:], pt[:], [pb], [sgb])
                    dma_add(fw, "sp", TOKB[r0:r0 + 128, gi * 512:(gi + 1) * 512], sg[:], reads=[sgb], wadd=[tokb_b])
                else:
                    fw.op("act", lambda e, pt=pt, sg=sg: e.copy(out=sg[:, 0:128], in_=pt[:, 0:128]), reads=[pb], writes=[sgb])
                    dma_add(fw, "sp", TOKB[r0:r0 + 128, 1536:1664], sg[:, 0:128], reads=[sgb], wadd=[tokb_b])
                    s32, s32b = stg32.next()
                    fw.op("act", lambda e, pt=pt, s32=s32: e.copy(out=s32[:, 0:12], in_=pt[:, 128:140]), reads=[pb], writes=[s32b])
                    dma_add(fw, "sp", TOKS[r0:r0 + 128, 0:12], s32[:, 0:12], reads=[s32b], wadd=[toks_b])

    fw_barrier(fw)
    ar.reset()

    if "fox" in parts:
        NH = 4
        flg = ar.alloc([NBLK, 4], F32); flg_b = Buf("flg")
        cum = ar.alloc([NBLK, 4], F32); cum_b = Buf("cum")
        tot = ar.alloc([NBLK, 4], F32); tot_b = Buf("tot")
        tab = ar.alloc([NH, NBLK, NBLK], F32); tab_b = Buf("tab")
        kT = [ar.alloc([S], BF16) for _ in range(2)]; kT_b = [Buf("kT0"), Buf("kT1")]
        qT = [ar.alloc([S], BF16) for _ in range(2)]; qT_b = [Buf("qT0"), Buf("qT1")]
        vp = [ar.alloc([NBLK, 130], BF16) for _ in range(2)]; vp_b = [Buf("vp0"), Buf("vp1")]
        pr = [ar.alloc([4, 128], BF16) for _ in range(3)]; pr_b = [Buf(f"pr{i}") for i in range(3)]
        rec = ar.alloc([4], F32); rec_b = Buf("rec")
        otk = [ar.alloc([128], BF16) for _ in range(2)]; otk_b = [Buf("otk0"), Buf("otk1")]
        pring = _R(pr, pr_b)
        oring = _R(otk, otk_b)
        for g8 in range(0, NBLK, 8):
            dma_add(fw, "sp", flg[:, g8:min(g8 + 8, NBLK), :], TOKS.rearrange("(blk p) c -> p blk c", p=128)[:, g8:min(g8 + 8, NBLK), 8:12], reads=[toks_b], wadd=[flg_b])
        fw.op("dve", lambda e: e.tensor_tensor(out=flg, in0=flg, in1=sm_t[:, 0:4].unsqueeze(1).broadcast_to([128, NBLK, 4]), op=ALU.add), reads=[flg_b, sm_b], writes=[flg_b])
        fw.op("act", lambda e: e.activation(out=flg, in_=flg, func=AF.Exp, scale=-1.0), reads=[flg_b], writes=[flg_b])
        fw.op("act", lambda e: e.activation(out=flg, in_=flg, func=AF.Ln, bias=1.0, scale=1.0), reads=[flg_b], writes=[flg_b])
        fw.op("dve", lambda e: e.tensor_scalar(out=flg, in0=flg, scalar1=-1.0, scalar2=None, op0=ALU.mult), reads=[flg_b], writes=[flg_b])
        flg2 = flg.rearrange("p a b -> p (a b)")
        pc, pcb = fw.psum()
        fw.op("pe", lambda e, pc=pc: e.matmul(pc[:, 0:NBLK * 4], U_f, flg2, start=True, stop=True), reads=[cf_b, flg_b], writes=[pcb])
        fw.op("dve", lambda e, pc=pc: e.tensor_copy(out=cum.rearrange("p a b -> p (a b)"), in_=pc[:, 0:NBLK * 4]), reads=[pcb], writes=[cum_b])
        pc2, pcb2 = fw.psum()
        fw.op("pe", lambda e, pc2=pc2: e.matmul(pc2[:, 0:NBLK * 4], ones_f, flg2, start=True, stop=True), reads=[cf_b, flg_b], writes=[pcb2])
        fw.op("dve", lambda e, pc2=pc2: e.tensor_copy(out=tot.rearrange("p a b -> p (a b)"), in_=pc2[:, 0:NBLK * 4]), reads=[pcb2], writes=[tot_b])
        for j in range(1, NBLK):
            fw.op("dve", lambda e, j=j: e.tensor_tensor(out=tot[:, j, :], in0=tot[:, j, :], in1=tot[:, j - 1, :], op=ALU.add), reads=[tot_b], writes=[tot_b])
        fw.op("dve", lambda e: e.tensor_tensor(out=cum[:, 1:NBLK, :], in0=cum[:, 1:NBLK, :], in1=tot[:, 0:NBLK - 1, :], op=ALU.add), reads=[tot_b, cum_b], writes=[cum_b])
        for h in range(NH):
            for i in range(NBLK):
                fw.op("dve", lambda e, h=h, i=i: e.tensor_scalar(out=tab[:, h, i, 0:i + 1], in0=cum[:, 0:i + 1, h], scalar1=-1.0, scalar2=tot[:, i, h:h + 1], op0=ALU.mult, op1=ALU.add), reads=[cum_b, tot_b], writes=[tab_b])
        oacc = fw.psum_reserve(2)
        otr = fw.psum_reserve(1)[0]
        SC = 128 ** -0.5
        for h in range(NH):
            kt_, ktb = kT[h % 2], kT_b[h % 2]
            qt_, qtb = qT[h % 2], qT_b[h % 2]
            vp_, vpb = vp[h % 2], vp_b[h % 2]
            fw.dma("sp", kt_, QKT[16 + h], reads=[qkt_b[16 + h]], writes=[ktb])
            fw.dma("sp", qt_, QKT[12 + h], reads=[qkt_b[12 + h]], writes=[qtb])
            fw.op("pool", lambda e, vp_=vp_: e.memset(vp_[:, :, 128:129], 1.0), writes=[vpb])
            for g8 in range(0, NBLK, 8):
                dma_add(fw, "sp", vp_[:, g8:min(g8 + 8, NBLK), 0:128], TOKB.rearrange("(blk p) c -> p blk c", p=128)[:, g8:min(g8 + 8, NBLK), h * 128:(h + 1) * 128], reads=[tokb_b, vpb], wadd=[vpb])
            for i in range(NBLK):
                oa, oab = oacc[i % 2]
                for j0 in range(0, i + 1, 4):
                    js = list(range(j0, min(j0 + 4, i + 1)))
                    ps, psb = fw.psum()
                    for jj, j in enumerate(js):
                        fw.op("pe", lambda e, jj=jj, j=j, i=i, ps=ps, kt_=kt_, qt_=qt_: e.matmul(ps[:, jj * 128:(jj + 1) * 128], kt_[:, j * 128:(j + 1) * 128], qt_[:, i * 128:(i + 1) * 128], start=True, stop=True), reads=[ktb, qtb], writes=[psb], sig=(jj == len(js) - 1))
                    p_, p_b = pring.next()
                    for jj, j in enumerate(js):
                        fw.op("act", lambda e, jj=jj, j=j, i=i, h=h, ps=ps, p_=p_: e.activation(out=p_[:, jj, :], in_=ps[:, jj * 128:(jj + 1) * 128], func=AF.Exp, scale=SC, bias=tab[:, h, i, j:j + 1]), reads=[psb, tab_b], writes=[p_b])
                        if j == i:
                            fw.op("pool", lambda e, jj=jj, p_=p_: e.tensor_tensor(out=p_[:, jj, :], in0=p_[:, jj, :], in1=mask_cur, op=ALU.mult), reads=[p_b, cb_b], writes=[p_b])
                    for jj, j in enumerate(js):
                        fw.op("pe", lambda e, jj=jj, j=j, i=i, oa=oa, p_=p_, vp_=vp_: e.matmul(oa[:, 0:129], p_[:, jj, :], vp_[:, j, 0:129], start=(j == 0), stop=(j == i)), reads=[p_b, vpb], writes=[oab], sig=(j == i or jj == len(js) - 1))
                fw.op("dve", lambda e, oa=oa, i=i: e.reciprocal(out=rec[:, i % 4:i % 4 + 1], in_=oa[:, 128:129]), reads=[oab], writes=[rec_b])
                ot_, ot_b = oring.next()
                fw.op("act", lambda e, oa=oa, i=i, ot_=ot_: e.activation(out=ot_, in_=oa[:, 0:128], func=AF.Copy, scale=rec[:, i % 4:i % 4 + 1]), reads=[oab, rec_b], writes=[ot_b])
                fw.op("pe", lambda e, i=i, ot_=ot_: e.matmul(otr[0][:, (i % 4) * 128:(i % 4 + 1) * 128], ot_, ident_bf, start=True, stop=True), reads=[ot_b, cb_b], writes=[otr[1]])
                if i % 4 == 3:
                    sg, sgb = stg.next()
                    evac(sg[:], otr[0][:], [otr[1]], [sgb])
                    fw.dma("sp", oT[512 + h * 128:512 + (h + 1) * 128, (i - 3) * 128:(i + 1) * 128], sg[:], reads=[sgb])
        fw.psum_release(oacc)
        fw.psum_release([otr])
        fw_barrier(fw)
        ar.reset()

    if "swa" in parts:
        import math
        TWO_PI = 2.0 * math.pi
        C1 = 6.28125
        C2 = TWO_PI - C1
        posi = ar.alloc([NBLK], I32); posf = ar.alloc([NBLK], F32); pos_b = Buf("pos")
        ang = ar.alloc([NBLK, 32], F32); ni = ar.alloc([NBLK, 32], I32); nf = ar.alloc([NBLK, 32], F32)
        rs = ar.alloc([NBLK, 32], F32); rc = ar.alloc([NBLK, 32], F32); mk = ar.alloc([NBLK, 32], F32)
        sinT = ar.alloc([NBLK, 32], F32); cosT = ar.alloc([NBLK, 32], F32)
        trig_b = Buf("trig")
        esink = ar.alloc([8], F32); esink_b = Buf("esink")
        invf = cf_t[:, CF_INVF:CF_INVF + 32]
        fw.dma("sp", posi, pos, writes=[pos_b])
        T_ = [trig_b, pos_b, cf_b]
        fw.op("dve", lambda e: e.tensor_copy(out=posf, in_=posi), reads=T_, writes=T_)
        fw.op("dve", lambda e: e.tensor_tensor(out=ang, in0=posf.unsqueeze(2).broadcast_to([128, NBLK, 32]), in1=invf.unsqueeze(1).broadcast_to([128, NBLK, 32]), op=ALU.mult), reads=T_, writes=T_)
        fw.op("dve", lambda e: e.tensor_scalar(out=ni, in0=ang, scalar1=1.0 / TWO_PI, scalar2=None, op0=ALU.mult), reads=T_, writes=T_)
        fw.op("dve", lambda e: e.tensor_copy(out=nf, in_=ni), reads=T_, writes=T_)
        fw.op("dve", lambda e: e.scalar_tensor_tensor(out=rs, in0=nf, scalar=-C1, in1=ang, op0=ALU.mult, op1=ALU.add), reads=T_, writes=T_)
        fw.op("dve", lambda e: e.scalar_tensor_tensor(out=rs, in0=nf, scalar=-C2, in1=rs, op0=ALU.mult, op1=ALU.add), reads=T_, writes=T_)

        def fix(r):
            fw.op("dve", lambda e: e.tensor_scalar(out=mk, in0=r, scalar1=math.pi, scalar2=None, op0=ALU.is_gt), reads=T_, writes=T_)
            fw.op("dve", lambda e: e.scalar_tensor_tensor(out=r, in0=mk, scalar=-TWO_PI, in1=r, op0=ALU.mult, op1=ALU.add), reads=T_, writes=T_)
            fw.op("dve", lambda e: e.tensor_scalar(out=mk, in0=r, scalar1=-math.pi, scalar2=None, op0=ALU.is_lt), reads=T_, writes=T_)
            fw.op("dve", lambda e: e.scalar_tensor_tensor(out=r, in0=mk, scalar=TWO_PI, in1=r, op0=ALU.mult, op1=ALU.add), reads=T_, writes=T_)
        fix(rs)
        fw.op("dve", lambda e: e.tensor_scalar(out=rc, in0=rs, scalar1=0.5 * math.pi, scalar2=None, op0=ALU.add), reads=T_, writes=T_)
        fix(rc)
        fw.op("act", lambda e: e.activation(out=sinT, in_=rs, func=AF.Sin), reads=T_, writes=T_)
        fw.op("act", lambda e: e.activation(out=cosT, in_=rc, func=AF.Sin), reads=T_, writes=T_)
        fw.op("act", lambda e: e.activation(out=esink, in_=sm_t[:, 4:12], func=AF.Exp), reads=[sm_b], writes=[esink_b])

        qkv = [ar.alloc([640], BF16) for _ in range(3)]; qkv_b = [Buf(f"qkv{i}") for i in range(3)]
        t14 = [ar.alloc([9, 32], F32) for _ in range(4)]; t14_b = Buf("t14")
        rot = [ar.alloc([9, 64], BF16) for _ in range(2)]; rot_b = [Buf("rot0"), Buf("rot1")]
        vpr = [ar.alloc([66], BF16) for _ in range(2)]; vpr_b = [Buf("vpr0"), Buf("vpr1")]
        kTb = [ar.alloc([128], BF16) for _ in range(2)]; kTb_b = [Buf("kTb0"), Buf("kTb1")]
        qTb = [ar.alloc([8, 128], BF16) for _ in range(2)]; qTb_b = [Buf("qTb0"), Buf("qTb1")]
        pp = [ar.alloc([4, 128], BF16) for _ in range(4)]; pp_b = [Buf(f"pp{i}") for i in range(4)]
        den = ar.alloc([8], F32); den_b = Buf("den")
        otok = [ar.alloc([8, 64], BF16) for _ in range(2)]; otok_b = [Buf("otok0"), Buf("otok1")]
        qring = _R(qkv, qkv_b)
        ppr = _R(pp, pp_b)
        for k_ in range(2):
            fw.op("pool", lambda e, k_=k_: e.memset(vpr[k_][:, 64:65], 1.0), writes=[vpr_b[k_]])
        TOKBv = TOKB.rearrange("(blk p) c -> blk p c", p=128)
        for blk in range(NBLK):
            qk_, qk_b = qring.next()
            fw.dma("sp", qk_, TOKBv[blk][:, 1024:1664], reads=[tokb_b], writes=[qk_b])
            q3 = qk_[:, 0:576].rearrange("p (h c) -> p h c", c=64)
            x1 = q3[:, :, 0:32]
            x2 = q3[:, :, 32:64]
            cb_ = cosT[:, blk, :].unsqueeze(1).broadcast_to([128, 9, 32])
            sb_ = sinT[:, blk, :].unsqueeze(1).broadcast_to([128, 9, 32])
            ro, rob = rot[blk % 2], rot_b[blk % 2]
            R_ = [qk_b, trig_b]
            fw.op("dve", lambda e, x1=x1, cb_=cb_: e.tensor_tensor(out=t14[0], in0=x1, in1=cb_, op=ALU.mult), reads=R_, writes=[t14_b])
            fw.op("dve", lambda e, x2=x2, sb_=sb_: e.tensor_tensor(out=t14[1], in0=x2, in1=sb_, op=ALU.mult), reads=R_, writes=[t14_b])
            fw.op("dve", lambda e, ro=ro: e.tensor_tensor(out=ro[:, :, 0:32], in0=t14[0], in1=t14[1], op=ALU.subtract), reads=[t14_b], writes=[rob, t14_b])
            fw.op("dve", lambda e, x2=x2, cb_=cb_: e.tensor_tensor(out=t14[2], in0=x2, in1=cb_, op=ALU.mult), reads=R_, writes=[t14_b])
            fw.op("dve", lambda e, x1=x1, sb_=sb_: e.tensor_tensor(out=t14[3], in0=x1, in1=sb_, op=ALU.mult), reads=R_, writes=[t14_b])
            fw.op("dve", lambda e, ro=ro: e.tensor_tensor(out=ro[:, :, 32:64], in0=t14[2], in1=t14[3], op=ALU.add), reads=[t14_b], writes=[rob, t14_b])
            vc, vcb = vpr[blk % 2], vpr_b[blk % 2]
            fw.op("pool", lambda e, vc=vc, qk_=qk_: e.tensor_copy(out=vc[:, 0:64], in_=qk_[:, 576:640]), reads=[qk_b], writes=[vcb])
            kc_, kcb = kTb[blk % 2], kTb_b[blk % 2]
            qc_, qcb = qTb[blk % 2], qTb_b[blk % 2]
            for half in range(2):
                pt, pb = fw.psum()
                for hl in range(4):
                    fw.op("pe", lambda e, hl=hl, half=half, pt=pt, ro=ro: e.matmul(pt[0:64, hl * 128:(hl + 1) * 128], ro[:, half * 4 + hl, :], ident_bf, start=True, stop=True), reads=[rob, cb_b], writes=[pb], sig=(hl == 3))
                evac(qc_[0:64, half * 4:(half + 1) * 4, :], pt[0:64, :].rearrange("p (h t) -> p h t", t=128), [pb], [qcb])
            pt, pb = fw.psum()
            fw.op("pe", lambda e, pt=pt, ro=ro: e.matmul(pt[0:64, 0:128], ro[:, 8, :], ident_bf, start=True, stop=True), reads=[rob, cb_b], writes=[pb])
            evac(kc_[0:64, :], pt[0:64, 0:128], [pb], [kcb])
            kbl = []
            if blk > 0:
                kbl.append((kTb[(blk - 1) % 2], kTb_b[(blk - 1) % 2], vpr[(blk - 1) % 2], vpr_b[(blk - 1) % 2], mask_prev))
            kbl.append((kc_, kcb, vc, vcb, mask_cur))
            oc, ocb = otok[blk % 2], otok_b[blk % 2]
            for half in range(2):
                pl = []
                for (kx, kxb, vx, vxb, mx) in kbl:
                    ps, psb = fw.psum()
                    fw.op("pe", lambda e, ps=ps, kx=kx, qc_=qc_, half=half: e.matmul(ps[:], kx[0:64, :], qc_[0:64, half * 4:(half + 1) * 4, :], start=True, stop=True), reads=[kxb, qcb], writes=[psb])
                    p_, p_b = ppr.next()
                    fw.op("act", lambda e, ps=ps, p_=p_: e.activation(out=p_, in_=ps[:].rearrange("p (h t) -> p h t", t=128), func=AF.Exp, scale=0.125), reads=[psb], writes=[p_b])
                    fw.op("pool", lambda e, p_=p_, mx=mx: e.tensor_tensor(out=p_, in0=p_, in1=mx.unsqueeze(1).broadcast_to([128, 4, 128]), op=ALU.mult), reads=[p_b, cb_b], writes=[p_b])
                    pl.append((p_, p_b, vx, vxb))
                oa, oab = fw.psum()
                for hl in range(4):
                    for ki, (p_, p_b, vx, vxb) in enumerate(pl):
                        fw.op("pe", lambda e, hl=hl, ki=ki, oa=oa, p_=p_, vx=vx, nk=len(pl): e.matmul(oa[:, hl * 65:hl * 65 + 65], p_[:, hl, :], vx[:, 0:65], start=(ki == 0), stop=(ki == nk - 1)), reads=[p_b, vxb], writes=[oab], sig=(ki == len(pl) - 1))
                oa3 = oa[:, 0:260].rearrange("p (h c) -> p h c", c=65)
                fw.op("dve", lambda e, oa3=oa3, half=half: e.tensor_tensor(out=den[:, half * 4:(half + 1) * 4], in0=oa3[:, :, 64], in1=esink[:, half * 4:(half + 1) * 4], op=ALU.add), reads=[oab, esink_b], writes=[den_b])
                fw.op("dve", lambda e, half=half: e.reciprocal(out=den[:, half * 4:(half + 1) * 4], in_=den[:, half * 4:(half + 1) * 4]), reads=[den_b], writes=[den_b])
                fw.op("dve", lambda e, oa3=oa3, half=half, oc=oc: e.tensor_tensor(out=oc[:, half * 4:(half + 1) * 4, :], in0=oa3[:, :, 0:64], in1=den[:, half * 4:(half + 1) * 4].unsqueeze(2).broadcast_to([128, 4, 64]), op=ALU.mult), reads=[oab, den_b], writes=[ocb])
            oc2 = oc.rearrange("p h c -> p (h c)")
            pt, pb = fw.psum()
            for c4 in range(4):
                fw.op("pe", lambda e, c4=c4, pt=pt, oc2=oc2: e.matmul(pt[:, c4 * 128:(c4 + 1) * 128], oc2[:, c4 * 128:(c4 + 1) * 128], ident_bf, start=True, stop=True), reads=[ocb, cb_b], writes=[pb], sig=(c4 == 3))
            sg, sgb = stg.next()
            evac(sg[:], pt[:], [pb], [sgb])
            fw.dma("sp", oT[1024:1536, blk * 128:(blk + 1) * 128].rearrange("(c p) t -> p c t", p=128), sg[:].rearrange("p (c t) -> p c t", t=128), reads=[sgb])
        fw_barrier(fw)
        ar.reset()

    if "dn" in parts:
        build_dn(nc, fw, ar, locals())

    fw.finish()
    fw.emit()
    return nc


def _fm(v):
    return np.ascontiguousarray(np.asarray(v, np.float32).reshape(KC, 128).T)


def host_vecs(inp, mods, layer, b):
    parts6 = np.split(mods[layer, b], 6)
    cols = [_fm(p) for p in parts6] + [_fm(inp["norm_mix"][layer]), _fm(inp["norm_ffn"][layer]), _fm(inp["final_norm"])]
    return np.ascontiguousarray(np.concatenate(cols, axis=1).astype(np.float32))


def host_consts():
    cf = np.zeros((128, CF_N), np.float32)
    idx = np.arange(128)
    cf[:, CF_ID:CF_ID + 128] = np.eye(128, dtype=np.float32)
    cf[:, CF_U:CF_U + 128] = (idx[:, None] <= idx[None, :]).astype(np.float32)
    cf[:, CF_ONES:CF_ONES + 128] = 1.0
    inv_freq = (10000.0 ** (-np.arange(0, 64, 2, dtype=np.float32) / np.float32(64))).astype(np.float32)
    cf[:, CF_INVF:CF_INVF + 32] = inv_freq[None, :]
    cf[:, CF_SL:CF_SL + 128] = (idx[:, None] > idx[None, :]).astype(np.float32)
    cf[:, CF_NEG1:CF_NEG1 + 128] = -1.0
    for k in range(7):
        b2 = 2 << k
        b1 = 1 << k
        m = ((idx[:, None] // b2) == (idx[None, :] // b2)) & ((idx[:, None] % b2) >= b1) & ((idx[None, :] % b2) < b1)
        cf[:, CF_LM + k * 128:CF_LM + (k + 1) * 128] = m.astype(np.float32)
    cb = np.zeros((128, CB_N), np.float32)
    cb[:, CB_ID:CB_ID + 128] = np.eye(128)
    cb[:, CB_MC:CB_MC + 128] = (idx[:, None] <= idx[None, :])
    cb[:, CB_MP:CB_MP + 128] = (idx[:, None] > idx[None, :])
    cb[:, CB_ONES:CB_ONES + 128] = 1.0
    return cf, cb.astype(ml_dtypes.bfloat16)


def host_inputs_A(inp, mods, layer, b, hh):
    w_in = inp["w_in"][layer]
    cols = []
    for base in (0, 1024, 2048, 4112, 5136):
        cols.append(np.arange(base + hh * 512, base + hh * 512 + 512))
    cols.append(np.arange(6160 + hh * 512, 6160 + hh * 512 + 512))
    cols.append(np.arange(3072 + hh * 512, 3072 + hh * 512 + 512))
    cols.append(np.arange(7192 + hh * 512, 7192 + hh * 512 + 512))
    last = np.concatenate([np.arange(8216 + hh * 64, 8216 + hh * 64 + 64), np.arange(8344 + hh * 64, 8344 + hh * 64 + 64),
                           np.arange(4096 + hh * 4, 4096 + hh * 4 + 4), np.arange(4104 + hh * 4, 4104 + hh * 4 + 4),
                           np.arange(7184 + hh * 4, 7184 + hh * 4 + 4)])
    wA = np.zeros((D, NCA * 512), np.float32)
    allc = np.concatenate(cols)
    wA[:, 0:allc.size] = w_in[:, allc]
    wA[:, 8 * 512:8 * 512 + last.size] = w_in[:, last]
    cf, cb = host_consts()
    sm = np.zeros((128, 64), np.float32)
    sm[:, 0:4] = inp["fox_b_forget"][layer][4 * hh:4 * hh + 4][None, :]
    sm[:, 4:12] = inp["swa_sinks"][layer][8 * hh:8 * hh + 8][None, :]
    sm[:, 12:16] = inp["dn_a_log"][layer][4 * hh:4 * hh + 4][None, :]
    sm[:, 16:20] = inp["dn_dt_bias"][layer][4 * hh:4 * hh + 4][None, :]
    dnw = np.zeros((128, 48 + 128), np.float32)
    cw = inp["conv_w"][layer]
    for kind in range(3):
        for hl in range(4):
            ch0 = kind * 1024 + (4 * hh + hl) * 128
            dnw[:, (kind * 4 + hl) * 4:(kind * 4 + hl) * 4 + 4] = cw[:, ch0:ch0 + 128].T
    dnw[:, 48:176] = inp["dn_norm"][layer][None, :]
    S = inp["x"].shape[1]
    xT_b = inp["_xT"][b] if "_xT" in inp else np.ascontiguousarray(inp["x"][b].T)
    return {"xT": xT_b, "vecs": host_vecs(inp, mods, layer, b), "wA": wA, "cf": cf, "cbf": cb,
            "pos": np.ascontiguousarray(inp["positions"][b].reshape(S // 128, 128).T.astype(np.int32)), "smallp": sm, "dnw": dnw}


def build_dn(nc, fw, ar, L):
    S, NBLK = L["S"], L["NBLK"]
    QKT, TOKB, TOKS, oT = L["QKT"], L["TOKB"], L["TOKS"], L["oT"]
    qkt_b, tokb_b, toks_b = L["qkt_b"], L["tokb_b"], L["toks_b"]
    sm_t, sm_b, dnw_t, dnw_b = L["sm_t"], L["sm_b"], L["dnw_t"], L["dnw_b"]
    cf_t, cf_b, cb_b = L["cf_t"], L["cf_b"], L["cb_b"]
    ident_bf, ones_bf, ident_f, U_f, ones_f = L["ident_bf"], L["ones_bf"], L["ident_f"], L["U_f"], L["ones_f"]
    stg, evac, _R = L["stg"], L["evac"], L["_R"]
    neg1_f = cf_t[:, CF_NEG1:CF_NEG1 + 128]
    SLm = cf_t[:, CF_SL:CF_SL + 128]
    NH = 4
    CW = min(512, S)

    def mm(out, lhsT, rhs, reads, writes, start=True, stop=True, sig=True):
        fw.op("pe", lambda e: e.matmul(out, lhsT, rhs, start=start, stop=stop), reads=reads, writes=writes, sig=sig)

    bet = ar.alloc([NBLK, 4], F32); g_ = ar.alloc([NBLK, 4], F32); gc = ar.alloc([NBLK, 4], F32)
    eg = ar.alloc([NBLK, 4], F32); ekd = ar.alloc([NBLK, 4], F32); gl = ar.alloc([NBLK, 4], F32)
    beg = ar.alloc([NBLK, 4], F32); ea = ar.alloc([4], F32)
    G = Buf("dn_gates")
    GR = [G, sm_b, cf_b]
    TOKSv = TOKS.rearrange("(blk p) c -> p blk c", p=128)
    for g8 in range(0, NBLK, 8):
        e8 = min(g8 + 8, NBLK)
        dma_add(fw, "sp", bet[:, g8:e8, :], TOKSv[:, g8:e8, 0:4], reads=[toks_b], wadd=[G])
        dma_add(fw, "sp", g_[:, g8:e8, :], TOKSv[:, g8:e8, 4:8], reads=[toks_b], wadd=[G])
    fw.op("act", lambda e: e.activation(out=bet, in_=bet, func=AF.Sigmoid), reads=GR, writes=[G])
    fw.op("dve", lambda e: e.tensor_tensor(out=g_, in0=g_, in1=sm_t[:, 16:20].unsqueeze(1).broadcast_to([128, NBLK, 4]), op=ALU.add), reads=GR, writes=[G])
    fw.op("act", lambda e: e.activation(out=g_, in_=g_, func=AF.Exp), reads=GR, writes=[G])
    fw.op("act", lambda e: e.activation(out=g_, in_=g_, func=AF.Ln, bias=1.0, scale=1.0), reads=GR, writes=[G])
    fw.op("act", lambda e: e.activation(out=ea, in_=sm_t[:, 12:16], func=AF.Exp), reads=GR, writes=[G])
    fw.op("dve", lambda e: e.tensor_tensor(out=g_, in0=g_, in1=ea.unsqueeze(1).broadcast_to([128, NBLK, 4]), op=ALU.mult), reads=GR, writes=[G])
    fw.op("dve", lambda e: e.tensor_scalar(out=g_, in0=g_, scalar1=-1.0, scalar2=None, op0=ALU.mult), reads=GR, writes=[G])
    g2 = g_.rearrange("p a b -> p (a b)")
    pc, pcb = fw.psum()
    mm(pc[:, 0:NBLK * 4], U_f, g2, GR, [pcb])
    fw.op("dve", lambda e: e.tensor_copy(out=gc.rearrange("p a b -> p (a b)"), in_=pc[:, 0:NBLK * 4]), reads=[pcb], writes=[G])
    pc2, pcb2 = fw.psum()
    mm(pc2[:, 0:NBLK * 4], ones_f, g2, GR, [pcb2])
    fw.op("dve", lambda e: e.tensor_copy(out=gl.rearrange("p a b -> p (a b)"), in_=pc2[:, 0:NBLK * 4]), reads=[pcb2], writes=[G])
    fw.op("dve", lambda e: e.tensor_tensor(out=ekd, in0=gl, in1=gc, op=ALU.subtract), reads=GR, writes=[G])
    fw.op("act", lambda e: e.activation(out=ekd, in_=ekd, func=AF.Exp), reads=GR, writes=[G])
    fw.op("act", lambda e: e.activation(out=gl, in_=gl, func=AF.Exp), reads=GR, writes=[G])
    fw.op("act", lambda e: e.activation(out=eg, in_=gc, func=AF.Exp), reads=GR, writes=[G])
    fw.op("dve", lambda e: e.tensor_tensor(out=beg, in0=bet, in1=eg, op=ALU.mult), reads=GR, writes=[G])

    if DN_STOP <= 1:
        return
    qn = [ar.alloc([S], BF16) for _ in range(NH)]
    kn = [ar.alloc([S], BF16) for _ in range(NH)]
    vs = [ar.alloc([S], BF16) for _ in range(NH)]
    QKV = [Buf(f"dn_qkv{h}") for h in range(NH)]
    mark = ar.off
    xpad = [ar.alloc([S + 4], BF16) for _ in range(2)]; xpad_b = [Buf("xpad0"), Buf("xpad1")]
    acc = ar.alloc([S], F32); acc_b = Buf("acc")
    sqc = ar.alloc([CW], BF16); sqc_b = Buf("sqc")
    rn = ar.alloc([CW], F32); rn_b = Buf("rn")
    for k_ in range(2):
        fw.op("dve", lambda e, k_=k_: e.memset(xpad[k_][:, 0:4], 0.0), writes=[xpad_b[k_]])
    cnt = 0
    for h in range(NH):
        for kind in range(3):
            xp, xpb = xpad[cnt % 2], xpad_b[cnt % 2]
            cnt += 1
            ch = kind * 4 + h
            fw.dma("sp", xp[:, 3:3 + S], QKT[ch], reads=[qkt_b[ch]], writes=[xpb])
            wc = lambda j, ch=ch: dnw_t[:, ch * 4 + j:ch * 4 + j + 1]
            fw.op("dve", lambda e, xp=xp, wc=wc: e.tensor_scalar(out=acc, in0=xp[:, 0:S], scalar1=wc(0), scalar2=None, op0=ALU.mult), reads=[xpb, dnw_b], writes=[acc_b])
            for j in range(1, 4):
                fw.op("dve", lambda e, xp=xp, wc=wc, j=j: e.scalar_tensor_tensor(out=acc, in0=xp[:, j:j + S], scalar=wc(j), in1=acc, op0=ALU.mult, op1=ALU.add), reads=[xpb, dnw_b, acc_b], writes=[acc_b])
            if kind == 2:
                fw.op("act", lambda e, h=h: e.activation(out=vs[h], in_=acc, func=AF.Silu), reads=[acc_b], writes=[QKV[h]])
                continue
            fw.op("act", lambda e: e.activation(out=acc, in_=acc, func=AF.Silu), reads=[acc_b], writes=[acc_b])
            dst = qn[h] if kind == 0 else kn[h]
            sc_ = (128 ** -0.5) if kind == 0 else 1.0
            for c0 in range(0, S, CW):
                fw.op("act", lambda e, c0=c0: e.activation(out=sqc, in_=acc[:, c0:c0 + CW], func=AF.Square), reads=[acc_b], writes=[sqc_b])
                pt, pb = fw.psum()
                mm(pt[:, 0:CW], ones_bf, sqc, [sqc_b, cb_b], [pb])
                fw.op("act", lambda e, pt=pt: e.activation(out=rn, in_=pt[:, 0:CW], func=AF.Sqrt, bias=EPS, scale=1.0), reads=[pb], writes=[rn_b])
                fw.op("dve", lambda e: e.reciprocal(out=rn, in_=rn), reads=[rn_b], writes=[rn_b])
                fw.op("dve", lambda e, c0=c0, dst=dst, sc_=sc_: e.scalar_tensor_tensor(out=dst[:, c0:c0 + CW], in0=acc[:, c0:c0 + CW], scalar=sc_, in1=rn, op0=ALU.mult, op1=ALU.mult), reads=[acc_b, rn_b], writes=[QKV[h]])
    fw_barrier(fw)
    ar.off = mark
    if DN_STOP <= 2:
        return

    def T128(dt=F32):
        return ar.alloc([128], dt)
    W = []
    for h in range(NH):
        w = dict(gU=T128(), dec=T128(), decT=T128(), Lm=T128(), PT=T128(), Ck=T128(), T=[T128(), T128()], TT=[T128(), T128()],
                 XT=T128(), u=T128(), wT=T128(), qdT=T128(), kbg=T128(), kd=T128(), vb=T128(), vnew=T128(), St=T128(),
                 zt=T128(BF16), zs=T128(), on=T128(), onb=T128(BF16), ss=ar.alloc([4], F32), junk=T128())
        w["b"] = {k: Buf(f"dn{h}_{k}") for k in ("gU", "dec", "decT", "Lm", "PT", "Ck", "T0", "T1", "TT0", "TT1", "XT", "u", "wT", "qdT", "kbg", "kd", "vb", "vnew", "St", "zt", "zs", "on", "onb", "ss", "junk")}
        W.append(w)
        fw.op("pool", lambda e, w=w: e.memset(w["St"], 0.0), writes=[w["b"]["St"]])
    otr = fw.psum_reserve(1)[0]
    TOKBv = TOKB.rearrange("(blk p) c -> blk p c", p=128)
    gain = dnw_t[:, 48:176]
    for c in range(NBLK):
        cs = slice(c * 128, (c + 1) * 128)
        for h in range(NH):
            w = W[h]; B = w["b"]
            Kt = kn[h][:, cs]; Qt = qn[h][:, cs]; Vt = vs[h][:, cs]
            gcol = lambda t_, c=c, h=h: t_[:, c, h:h + 1]
            pt, pb = fw.psum()
            mm(pt[:, 0:128], Kt, ident_bf, [QKV[h], cb_b], [pb])
            fw.op("act", lambda e, pt=pt, w=w, gcol=gcol: e.activation(out=w["kbg"], in_=pt[:, 0:128], func=AF.Copy, scale=gcol(beg)), reads=[pb, G], writes=[B["kbg"]])
            fw.op("act", lambda e, pt=pt, w=w, gcol=gcol: e.activation(out=w["kd"], in_=pt[:, 0:128], func=AF.Copy, scale=gcol(ekd)), reads=[pb, G], writes=[B["kd"]])
            mm(pt[:, 128:256], Vt, ident_bf, [QKV[h], cb_b], [pb])
            fw.op("act", lambda e, pt=pt, w=w, gcol=gcol: e.activation(out=w["vb"], in_=pt[:, 128:256], func=AF.Copy, scale=gcol(bet)), reads=[pb, G], writes=[B["vb"]])
            fw.op("dve", lambda e, w=w, gcol=gcol: e.tensor_scalar(out=w["gU"], in0=U_f, scalar1=gcol(g_), scalar2=None, op0=ALU.mult), reads=[cf_b, G], writes=[B["gU"]])
            pd, pdb = fw.psum()
            mm(pd[:, 0:128], w["gU"], ones_f, [B["gU"], cf_b], [pdb], start=True, stop=False, sig=False)
            mm(pd[:, 0:128], neg1_f, w["gU"], [B["gU"], cf_b], [pdb], start=False, stop=True, sig=False)
            mm(pd[:, 128:256], ones_f, w["gU"], [B["gU"], cf_b], [pdb], start=True, stop=False, sig=False)
            mm(pd[:, 128:256], w["gU"], neg1_f, [B["gU"], cf_b], [pdb], start=False, stop=True, sig=False)
            mm(pd[:, 256:384], ones_f, w["gU"], [B["gU"], cf_b], [pdb], start=True, stop=True, sig=True)
            fw.op("dve", lambda e, pd=pd, w=w: e.tensor_scalar(out=w["dec"], in0=pd[:, 0:128], scalar1=0.0, scalar2=None, op0=ALU.min), reads=[pdb], writes=[B["dec"]])
            fw.op("act", lambda e, w=w: e.activation(out=w["dec"], in_=w["dec"], func=AF.Exp), reads=[B["dec"]], writes=[B["dec"]])
            fw.op("dve", lambda e, pd=pd, w=w: e.tensor_scalar(out=w["decT"], in0=pd[:, 128:256], scalar1=0.0, scalar2=None, op0=ALU.min), reads=[pdb], writes=[B["decT"]])
            fw.op("act", lambda e, w=w: e.activation(out=w["decT"], in_=w["decT"], func=AF.Exp), reads=[B["decT"]], writes=[B["decT"]])
            fw.op("dve", lambda e, pd=pd, w=w: e.tensor_scalar(out=w["junk"], in0=pd[:, 256:384], scalar1=0.0, scalar2=None, op0=ALU.min), reads=[pdb], writes=[B["junk"]])
            fw.op("act", lambda e, w=w: e.activation(out=w["junk"], in_=w["junk"], func=AF.Exp), reads=[B["junk"]], writes=[B["junk"]])
            fw.op("dve", lambda e, w=w, Qt=Qt: e.tensor_tensor(out=w["qdT"], in0=Qt, in1=w["junk"], op=ALU.mult), reads=[QKV[h], B["junk"]], writes=[B["qdT"]])
            pa, pab = fw.psum()
            mm(pa[:, 0:128], Kt, Kt, [QKV[h]], [pab], sig=False)
            mm(pa[:, 128:256], Kt, Qt, [QKV[h]], [pab])
            fw.op("dve", lambda e, pa=pa, w=w, gcol=gcol: e.scalar_tensor_tensor(out=w["Lm"], in0=pa[:, 0:128], scalar=gcol(bet), in1=w["dec"], op0=ALU.mult, op1=ALU.mult), reads=[pab, G, B["dec"]], writes=[B["Lm"]])
            fw.op("pool", lambda e, w=w: e.tensor_tensor(out=w["Lm"], in0=w["Lm"], in1=SLm, op=ALU.mult), reads=[B["Lm"], cf_b], writes=[B["Lm"]])
            fw.op("dve", lambda e, pa=pa, w=w: e.tensor_tensor(out=w["PT"], in0=pa[:, 128:256], in1=w["decT"], op=ALU.mult), reads=[pab, B["decT"]], writes=[B["PT"]])
            fw.op("pool", lambda e, w=w: e.tensor_tensor(out=w["PT"], in0=w["PT"], in1=U_f, op=ALU.mult), reads=[B["PT"], cf_b], writes=[B["PT"]])
            if DN_STOP <= 3:
                continue
            cur = 0
            fw.op("pool", lambda e, w=w: e.tensor_copy(out=w["T"][0], in_=ident_f), reads=[cf_b], writes=[B["T0"]])
            fw.op("pool", lambda e, w=w: e.tensor_copy(out=w["TT"][0], in_=ident_f), reads=[cf_b], writes=[B["TT0"]])
            for k in range(7):
                Tc, TTc = w["T"][cur], w["TT"][cur]
                Tn, TTn = w["T"][1 - cur], w["TT"][1 - cur]
                bTc, bTTc, bTn, bTTn = B[f"T{cur}"], B[f"TT{cur}"], B[f"T{1 - cur}"], B[f"TT{1 - cur}"]
                lmk = cf_t[:, CF_LM + k * 128:CF_LM + (k + 1) * 128]
                fw.op("pool", lambda e, w=w, lmk=lmk: e.tensor_tensor(out=w["Ck"], in0=w["Lm"], in1=lmk, op=ALU.mult), reads=[B["Lm"], cf_b], writes=[B["Ck"]])
                px, pxb = fw.psum()
                mm(px[:, 0:128], w["Ck"], TTc, [B["Ck"], bTTc], [pxb])
                fw.op("act", lambda e, px=px, w=w: e.copy(out=w["XT"], in_=px[:, 0:128]), reads=[pxb], writes=[B["XT"]])
                py, pyb = fw.psum()
                mm(py[:, 0:128], Tc, w["XT"], [bTc, B["XT"]], [pyb], sig=(k == 6))
                if k < 6:
                    mm(py[:, 128:256], w["XT"], Tc, [bTc, B["XT"]], [pyb])
                fw.op("dve", lambda e, py=py, TTc=TTc, TTn=TTn: e.tensor_tensor(out=TTn, in0=TTc, in1=py[:, 0:128], op=ALU.subtract), reads=[pyb, bTTc], writes=[bTTn])
                if k < 6:
                    fw.op("dve", lambda e, py=py, Tc=Tc, Tn=Tn: e.tensor_tensor(out=Tn, in0=Tc, in1=py[:, 128:256], op=ALU.subtract), reads=[pyb, bTc], writes=[bTn])
                cur = 1 - cur
            if DN_STOP <= 4:
                continue
            TT = w["TT"][cur]; bTT = B[f"TT{cur}"]
            pu, pub = fw.psum()
            mm(pu[:, 0:128], TT, w["vb"], [bTT, B["vb"]], [pub], sig=False)
            mm(pu[:, 128:256], w["kbg"], TT, [bTT, B["kbg"]], [pub])
            if DN_X >= 2:
                fw.op("act", lambda e, pu=pu, w=w: e.copy(out=w["u"], in_=pu[:, 0:128]), reads=[pub], writes=[B["u"]])
            if DN_X >= 3:
                fw.op("act", lambda e, pu=pu, w=w: e.copy(out=w["wT"], in_=pu[:, 128:256]), reads=[pub], writes=[B["wT"]])
            if DN_STOP <= 4.5:
                continue
            fw.dma("sp", w["zt"], TOKBv[c][:, 512 + h * 128:512 + (h + 1) * 128], reads=[tokb_b], writes=[B["zt"]])
            fw.op("act", lambda e, w=w: e.activation(out=w["zs"], in_=w["zt"], func=AF.Silu), reads=[B["zt"]], writes=[B["zs"]])
        if DN_STOP <= 5:
            continue
        for h in range(NH):
            w = W[h]; B = w["b"]
            p1, p1b = fw.psum()
            mm(p1[:, 0:128], w["wT"], w["St"], [B["wT"], B["St"]], [p1b])
            fw.op("dve", lambda e, p1=p1, w=w: e.tensor_tensor(out=w["vnew"], in0=w["u"], in1=p1[:, 0:128], op=ALU.subtract), reads=[p1b, B["u"]], writes=[B["vnew"]])
            p2, p2b = fw.psum()
            mm(p2[:, 0:128], w["qdT"], w["St"], [B["qdT"], B["St"]], [p2b], start=True, stop=False, sig=False)
            mm(p2[:, 0:128], w["PT"], w["vnew"], [B["PT"], B["vnew"]], [p2b], start=False, stop=True, sig=True)
            p3, p3b = fw.psum()
            mm(p3[:, 0:128], w["kd"], w["vnew"], [B["kd"], B["vnew"]], [p3b])
            fw.op("dve", lambda e, p3=p3, w=w, c=c, h=h: e.scalar_tensor_tensor(out=w["St"], in0=w["St"], scalar=gl[:, c, h:h + 1], in1=p3[:, 0:128], op0=ALU.mult, op1=ALU.add), reads=[p3b, G, B["St"]], writes=[B["St"]])
            fw.op("act", lambda e, p2=p2, w=w: e.activation(out=w["junk"], in_=p2[:, 0:128], func=AF.Square, accum_out=w["ss"][:, 0:1]), reads=[p2b], writes=[B["junk"], B["ss"]])
            fw.op("act", lambda e, w=w: e.activation(out=w["ss"][:, 1:2], in_=w["ss"][:, 0:1], func=AF.Sqrt, scale=1.0 / 128, bias=EPS), reads=[B["ss"]], writes=[B["ss"]])
            fw.op("dve", lambda e, w=w: e.reciprocal(out=w["ss"][:, 2:3], in_=w["ss"][:, 1:2]), reads=[B["ss"]], writes=[B["ss"]])
            fw.op("act", lambda e, p2=p2, w=w: e.activation(out=w["on"], in_=p2[:, 0:128], func=AF.Copy, scale=w["ss"][:, 2:3]), reads=[p2b, B["ss"]], writes=[B["on"]])
            fw.op("pool", lambda e, w=w: e.tensor_tensor(out=w["zs"], in0=w["zs"], in1=gain, op=ALU.mult), reads=[B["zs"], dnw_b], writes=[B["zs"]])
            fw.op("pool", lambda e, w=w: e.tensor_tensor(out=w["onb"], in0=w["on"], in1=w["zs"], op=ALU.mult), reads=[B["on"], B["zs"]], writes=[B["onb"]])
            mm(otr[0][:, h * 128:(h + 1) * 128], w["onb"], ident_bf, [B["onb"], cb_b], [otr[1]], sig=(h == NH - 1))
        sg, sgb = stg.next()
        evac(sg[:], otr[0][:], [otr[1]], [sgb])
        fw.dma("sp", oT[0:512, c * 128:(c + 1) * 128].rearrange("(h p) t -> p h t", p=128), sg[:].rearrange("p (h t) -> p h t", t=128), reads=[sgb])
    fw.psum_release([otr])


MCOL = 3072


def build_M():
    nc = bass.Bass("TRN2", target_bir_lowering=False)
    fw = FW(nc)
    fw.init_psum()
    cT = nc.dram_tensor("cT", [128, KC * NB], F32, kind="ExternalInput").ap()
    wad = nc.dram_tensor("wad", [D, MCOL], F32, kind="ExternalInput").ap()
    bias = nc.dram_tensor("bias", [NB, MCOL], F32, kind="ExternalInput").ap()
    modo = nc.dram_tensor("modo", [NB, MCOL], F32, kind="ExternalOutput").ap()
    sb = nc.alloc_sbuf_tensor
    ca = sb("ca", [128, KC * NB], F32); ca_b = Buf("ca")
    bt = sb("bt", [NB, MCOL], F32); bt_b = Buf("bt")
    rt = sb("rt", [NB, MCOL], F32); rt_b = Buf("rt")
    wr = Ring(nc, "wm", 3, [128, KC, 512], F32)
    fw.dma("sp", ca[:], cT, writes=[ca_b])
    fw.dma("sp", bt[:], bias, writes=[bt_b])
    fw.op("act", lambda e: e.activation(out=ca[:], in_=ca[:], func=AF.Silu), reads=[ca_b], writes=[ca_b])
    wv = wad.rearrange("(kc p) c -> p kc c", p=128)
    for ct in range(MCOL // 512):
        wt, wb = wr.next()
        fw.dma("sp", wt[:, 0:8, :], wv[:, 0:8, ct * 512:(ct + 1) * 512], writes=[wb])
        tk2 = dma_add(fw, "sp", wt[:, 8:16, :], wv[:, 8:16, ct * 512:(ct + 1) * 512], reads=[wb], wadd=[wb])
        pt, pb = fw.psum()
        for kc in range(KC):
            fw.op("pe", lambda e, kc=kc, pt=pt, wt=wt: e.matmul(pt[0:NB, :], ca[:, kc * NB:(kc + 1) * NB], wt[:, kc, :], start=(kc == 0), stop=(kc == KC - 1)), reads=[wb, ca_b], writes=[pb], sig=(kc == KC - 1))
        fw.op("dve", lambda e, ct=ct, pt=pt: e.tensor_tensor(out=rt[:, ct * 512:(ct + 1) * 512], in0=pt[0:NB, :], in1=bt[:, ct * 512:(ct + 1) * 512], op=ALU.add), reads=[pb, bt_b], writes=[rt_b])
    fw.dma("sp", modo, rt[:], reads=[rt_b])
    fw.finish()
    fw.emit()
    return nc


def _run(nc, in_maps):
    res = run_bass_kernel_spmd(nc, in_maps, core_ids=list(range(len(in_maps))))
    return res.results


def kernel(**inp):
    inp = {k: np.asarray(v) for k, v in inp.items()}
    NCORE = 8
    c = inp["c"].astype(np.float32)
    cT = np.ascontiguousarray(c.T.reshape(KC, 128, NB).transpose(1, 0, 2).reshape(128, KC * NB))
    ims = []
    for i in range(NCORE):
        l, q = i // 4, i % 4
        ims.append({"cT": cT, "wad": np.ascontiguousarray(inp["w_ada"][l][:, q * MCOL:(q + 1) * MCOL]),
                    "bias": np.ascontiguousarray(np.broadcast_to(inp["b_ada"][l][q * MCOL:(q + 1) * MCOL][None, :], (NB, MCOL)))})
    rs = _run(build_M(), ims)
    mods = np.zeros((2, NB, 6 * D), np.float32)
    for i in range(NCORE):
        l, q = i // 4, i % 4
        mods[l, :, q * MCOL:(q + 1) * MCOL] = rs[i]["modo"]
    xT = [np.ascontiguousarray(inp["x"][b].T) for b in range(NB)]
    S = inp["x"].shape[1]
    T = S // 2
    ncA = build_A(S=S)
    sel = np.zeros((NEXP, NEXP * 128), np.float32)
    for e in range(NEXP):
        sel[e, e * 128:(e + 1) * 128] = 1.0
    ident = np.eye(128, dtype=np.float32)
    for layer in range(2):
        inp["_xT"] = xT
        ims = [host_inputs_A(inp, mods, layer, i // 2, i % 2) for i in range(NCORE)]
        rs = _run(ncA, ims)
        moe = (layer % 2 == 1)
        final = (layer == 1)
        ncB = build_B(T=T, moe=moe, final=final)
        w_gate = np.ascontiguousarray(inp["w_in"][layer][:, GATE_OFF:])
        w_branch = np.ascontiguousarray(inp["w_branch"][layer].reshape(3 * 1024, D))
        ims = []
        for i in range(NCORE):
            b, th = i // 2, i % 2
            o0, o1 = rs[2 * b]["oT"], rs[2 * b + 1]["oT"]
            oT = np.empty((3 * 1024, T), dtype=o0.dtype)
            for r in range(3):
                oT[r * 1024:r * 1024 + 512] = o0[r * 512:(r + 1) * 512, th * T:(th + 1) * T]
                oT[r * 1024 + 512:(r + 1) * 1024] = o1[r * 512:(r + 1) * 512, th * T:(th + 1) * T]
            im = {"xT": np.ascontiguousarray(xT[b][:, th * T:(th + 1) * T]), "vecs": host_vecs(inp, mods, layer, b), "oT": oT,
                  "w_gate": w_gate, "w_branch": w_branch, "w_out": inp["w_out"][layer]}
            if moe:
                im.update({"wg": inp["moe_w_gate"][0], "wu": inp["moe_w_up"][0], "wd": inp["moe_w_down"][0],
                           "w_router": inp["moe_router"][0], "c_ident": ident, "c_sel": sel})
            else:
                im.update({"wg": inp["ffn_w_gate"][0:1], "wu": inp["ffn_w_up"][0:1], "wd": inp["ffn_w_down"][0:1]})
            ims.append(im)
        rs = _run(ncB, ims)
        for i in range(NCORE):
            b, th = i // 2, i % 2
            xT[b][:, th * T:(th + 1) * T] = rs[i]["yT"]
    out = np.stack([np.ascontiguousarray(xT[b].T) for b in range(NB)], axis=0).astype(np.float32)
    return out
```
